# Optimizing a Trainium2 kernel written in Bass

```python
import math
import jax, jax.numpy as jnp
from jax import lax
import numpy as np

D_MODEL = 4096
BATCH = 4
SEQ = 4096
DEPTH = 2

HEAD_DIM = 128
MIX_WIDTH = D_MODEL
ATT_HEADS = (3 * MIX_WIDTH) // (8 * HEAD_DIM)
ATT_KV_HEADS = ATT_HEADS // 3
WINDOW = 128
ATT_BLOCK = WINDOW
RET_HEADS = (3 * MIX_WIDTH) // (8 * HEAD_DIM)
RET_CHUNK = 128
CONV_CH = MIX_WIDTH - (ATT_HEADS + RET_HEADS) * HEAD_DIM
CONV_WIDTH = 31
D_FF = 11008
FFN_CONV_WIDTH = 3

ATT_Q_COLS = ATT_HEADS * HEAD_DIM
ATT_KV_COLS = ATT_KV_HEADS * HEAD_DIM
CONV_IN_COLS = 2 * CONV_CH
RET_COLS = RET_HEADS * HEAD_DIM
IN_COLS = ATT_Q_COLS + 2 * ATT_KV_COLS + CONV_IN_COLS + 4 * RET_COLS

NORM_EPS = 1e-6
NEG_INF = -1e30

kernel_name = "hybrid_swa_conformer_retention_convffn"


def rmsnorm(x, g):
    xf = x.astype(jnp.float32)
    y = xf * lax.rsqrt(jnp.mean(xf * xf, axis=-1, keepdims=True) + NORM_EPS)
    return (y * g.astype(jnp.float32)).astype(x.dtype)


def layernorm(x, g, b):
    xf = x.astype(jnp.float32)
    mu = jnp.mean(xf, axis=-1, keepdims=True)
    xc = xf - mu
    y = xc * lax.rsqrt(jnp.mean(xc * xc, axis=-1, keepdims=True) + NORM_EPS)
    return (y * g.astype(jnp.float32) + b.astype(jnp.float32)).astype(x.dtype)


def causal_depthwise_conv(x, w, b):
    kw, c = w.shape
    y = lax.conv_general_dilated(
        x, w[:, None, :].astype(x.dtype), window_strides=(1,), padding=[(kw - 1, 0)],
        dimension_numbers=("NWC", "WIO", "NWC"), feature_group_count=c)
    return y + b.astype(x.dtype)


def _pow2_slopes(n):
    start = 2.0 ** (-8.0 / n)
    return [start ** (i + 1) for i in range(n)]


def alibi_slopes(n):
    if math.log2(n).is_integer():
        return _pow2_slopes(n)
    c = 2 ** math.floor(math.log2(n))
    return _pow2_slopes(c) + alibi_slopes(2 * c)[0::2][: n - c]


def sliding_window_attention(q, k, v, sinks, slopes):
    b, s, h, d = q.shape
    hkv = k.shape[2]
    g = h // hkv
    nb = s // ATT_BLOCK
    qb = q.reshape(b, nb, ATT_BLOCK, hkv, g, d)

    def with_prev(t):
        tb = t.reshape(b, nb, ATT_BLOCK, hkv, d)
        prev = jnp.pad(tb, ((0, 0), (1, 0), (0, 0), (0, 0), (0, 0)))[:, :-1]
        return jnp.concatenate([prev, tb], axis=2)

    kc, vc = with_prev(k), with_prev(v)
    scores = jnp.einsum("bnqhgd,bnkhd->bnhgqk", qb, kc).astype(jnp.float32) * (d ** -0.5)
    qi = jnp.arange(ATT_BLOCK)[:, None]
    kj = jnp.arange(2 * ATT_BLOCK)[None, :]
    dist = qi + ATT_BLOCK - kj
    in_window = (dist >= 0) & (dist < WINDOW)
    key_exists = ~((jnp.arange(nb)[:, None, None] == 0) & (kj < ATT_BLOCK)[None])
    valid = in_window[None] & key_exists
    slopes_hg = slopes.reshape(hkv, g).astype(jnp.float32)
    scores = scores - slopes_hg[:, :, None, None] * dist.astype(jnp.float32)
    scores = jnp.where(valid[None, :, None, None], scores, NEG_INF)
    sink = jnp.broadcast_to(sinks.reshape(hkv, g).astype(jnp.float32)[None, None, :, :, None, None],
                            scores.shape[:-1] + (1,))
    probs = jax.nn.softmax(jnp.concatenate([scores, sink], axis=-1), axis=-1)[..., :-1]
    out = jnp.einsum("bnhgqk,bnkhd->bnqhgd", probs.astype(v.dtype), vc)
    return out.reshape(b, s, h * d)


def retention(q, k, v, log_gamma):
    b, s, h, d = q.shape
    n = s // RET_CHUNK
    qc = q.astype(jnp.float32).reshape(b, n, RET_CHUNK, h, d)
    kc = k.astype(jnp.float32).reshape(b, n, RET_CHUNK, h, d) * (d ** -0.5)
    vc = v.astype(jnp.float32).reshape(b, n, RET_CHUNK, h, d)
    pos = jnp.arange(RET_CHUNK, dtype=jnp.float32)
    rel = pos[:, None] - pos[None, :]
    intra_decay = jnp.where(rel >= 0, jnp.exp(log_gamma[:, None, None] * jnp.maximum(rel, 0.0)), 0.0)
    scores = jnp.einsum("bnihd,bnjhd->bnhij", qc, kc) * intra_decay
    intra = jnp.einsum("bnhij,bnjhd->bnihd", scores, vc)
    q_decay = jnp.exp(log_gamma[:, None] * (pos + 1.0)[None])
    k_decay = jnp.exp(log_gamma[:, None] * (RET_CHUNK - 1.0 - pos)[None])
    chunk_decay = jnp.exp(log_gamma * RET_CHUNK)
    kv = jnp.einsum("bnjhd,bnjhe,hj->nbhde", kc, vc, k_decay)

    def step(state, kv_n):
        return state * chunk_decay[None, :, None, None] + kv_n, state

    _, prev = lax.scan(step, jnp.zeros((b, h, d, d), jnp.float32), kv)
    cross = jnp.einsum("bnihd,nbhde,hi->bnihe", qc, prev, q_decay)
    return (intra + cross).reshape(b, s, h, d)


def head_groupnorm(x):
    mu = jnp.mean(x, axis=-1, keepdims=True)
    xc = x - mu
    return xc * lax.rsqrt(jnp.mean(xc * xc, axis=-1, keepdims=True) + NORM_EPS)


def conformer_conv(u, dw_w, dw_b, ln_g, ln_b, pw_w):
    a, gate = jnp.split(u, 2, axis=-1)
    y = a * jax.nn.sigmoid(gate)
    y = causal_depthwise_conv(y, dw_w, dw_b)
    y = layernorm(y, ln_g, ln_b)
    y = jax.nn.silu(y)
    return y @ pw_w


def conv_ffn(h, w_in, dw_w, dw_b, w_out):
    u = causal_depthwise_conv(h @ w_in, dw_w, dw_b)
    gate, val = jnp.split(u, 2, axis=-1)
    return (jax.nn.gelu(gate, approximate=True) * val) @ w_out


def setup_inputs(seed: int = 0) -> dict:
    key = jax.random.key(seed)
    ks = jax.random.split(key, 20)
    L = DEPTH

    def nrm(k, shape, scale):
        return jax.random.normal(k, shape, jnp.float32) * scale

    def gain(k, shape):
        return 1.0 + 0.1 * jax.random.normal(k, shape, jnp.float32)

    return {
        "x": nrm(ks[0], (BATCH, SEQ, D_MODEL), 1.0),
        "mix_pre_g": gain(ks[1], (L, D_MODEL)),
        "mix_post_g": gain(ks[2], (L, D_MODEL)),
        "ffn_pre_g": gain(ks[3], (L, D_MODEL)),
        "ffn_post_g": gain(ks[4], (L, D_MODEL)),
        "w_in": nrm(ks[5], (L, D_MODEL, IN_COLS), D_MODEL ** -0.5),
        "att_sinks": nrm(ks[6], (L, ATT_HEADS), 0.5),
        "conv_dw_w": nrm(ks[7], (L, CONV_WIDTH, CONV_CH), CONV_WIDTH ** -0.5),
        "conv_dw_b": nrm(ks[8], (L, CONV_CH), 0.02),
        "conv_ln_g": gain(ks[9], (L, CONV_CH)),
        "conv_ln_b": nrm(ks[10], (L, CONV_CH), 0.02),
        "conv_pw_w": nrm(ks[11], (L, CONV_CH, CONV_CH), CONV_CH ** -0.5),
        "w_out": nrm(ks[12], (L, MIX_WIDTH, D_MODEL), MIX_WIDTH ** -0.5),
        "ffn_w_in": nrm(ks[13], (L, D_MODEL, 2 * D_FF), D_MODEL ** -0.5),
        "ffn_dw_w": nrm(ks[14], (L, FFN_CONV_WIDTH, 2 * D_FF), FFN_CONV_WIDTH ** -0.5),
        "ffn_dw_b": nrm(ks[15], (L, 2 * D_FF), 0.02),
        "ffn_w_out": nrm(ks[16], (L, D_FF, D_MODEL), D_FF ** -0.5),
    }


def reference(x, mix_pre_g, mix_post_g, ffn_pre_g, ffn_post_g, w_in, att_sinks,
              conv_dw_w, conv_dw_b, conv_ln_g, conv_ln_b, conv_pw_w, w_out,
              ffn_w_in, ffn_dw_w, ffn_dw_b, ffn_w_out):
    b, s, _ = x.shape
    slopes = jnp.asarray(alibi_slopes(ATT_HEADS), dtype=jnp.float32)
    log_gamma = jnp.log1p(-jnp.exp2(-5.0 - jnp.arange(RET_HEADS, dtype=jnp.float32)))
    split_points = list(np.cumsum([ATT_Q_COLS, ATT_KV_COLS, ATT_KV_COLS, CONV_IN_COLS,
                                   RET_COLS, RET_COLS, RET_COLS]))
    for l in range(DEPTH):
        h = rmsnorm(x, mix_pre_g[l])
        proj = h @ w_in[l]
        qa, ka, va, cu, qr, kr, vr, gr = jnp.split(proj, split_points, axis=-1)
        att = sliding_window_attention(
            qa.reshape(b, s, ATT_HEADS, HEAD_DIM),
            ka.reshape(b, s, ATT_KV_HEADS, HEAD_DIM),
            va.reshape(b, s, ATT_KV_HEADS, HEAD_DIM),
            att_sinks[l], slopes)
        conv = conformer_conv(cu, conv_dw_w[l], conv_dw_b[l], conv_ln_g[l], conv_ln_b[l],
                              conv_pw_w[l])
        ret = retention(qr.reshape(b, s, RET_HEADS, HEAD_DIM),
                        kr.reshape(b, s, RET_HEADS, HEAD_DIM),
                        vr.reshape(b, s, RET_HEADS, HEAD_DIM), log_gamma)
        ret = head_groupnorm(ret).reshape(b, s, RET_COLS)
        ret = (ret * jax.nn.silu(gr.astype(jnp.float32))).astype(x.dtype)
        mixed = jnp.concatenate([att.astype(x.dtype), conv.astype(x.dtype), ret], axis=-1) @ w_out[l]
        x = x + rmsnorm(mixed, mix_post_g[l])
        h = rmsnorm(x, ffn_pre_g[l])
        x = x + rmsnorm(conv_ffn(h, ffn_w_in[l], ffn_dw_w[l], ffn_dw_b[l], ffn_w_out[l]), ffn_post_g[l])
    return x
```

```python
import math
import numpy as np
import concourse.bass as bass
import concourse.mybir as mybir
from concourse.bass_utils import run_bass_kernel_spmd

F32 = mybir.dt.float32
BF16 = mybir.dt.bfloat16
AF = mybir.ActivationFunctionType
ALU = mybir.AluOpType
AX = mybir.AxisListType

D = 4096
KC = 32
NCORE = 8
TT = 512
HD = 128
NH = 12
NKV = 4
CONVC = 1024
CW = 31
DFF = 11008
NFB = 86
QSZ = [22, 22, 21, 21]
QKC = 22
EPS = 1e-6
NEG = -30000.0
NPAR = 1100
NCON = 3480
SELFSYNC = True


def _slopes(n):
    def p2(n):
        s = 2.0 ** (-8.0 / n)
        return [s ** (i + 1) for i in range(n)]
    if math.log2(n).is_integer():
        return p2(n)
    c = 2 ** math.floor(math.log2(n))
    return p2(c) + _slopes(2 * c)[0::2][: n - c]


class Buf:
    def __init__(self, name, dsem=None):
        self.name = name
        self.w = None
        self.r = {}
        self.dsem = dsem
        self.dcnt = 0


class Eng:
    def __init__(self, name, k, selfsync):
        self.name = name
        self.k = k
        self.n = 0
        self.waited = {}
        self.selfsync = selfsync
        self.prog = []


class Ctx:
    def __init__(self, nc):
        self.nc = nc
        self.sems = []
        self.owner = {}
        self.engs = {}

    def new_sem(self, name):
        s = self.nc.alloc_semaphore(name)
        self.sems.append(s)
        return len(self.sems) - 1

    def add_engine(self, name, selfsync):
        k = self.new_sem("p_" + name)
        e = Eng(name, k, selfsync)
        self.owner[k] = e
        self.engs[name] = e
        return e

    def dbuf(self, name):
        return Buf(name, self.new_sem("d_" + name))

    def _waits(self, E, deps):
        for k, v in deps.items():
            if k == E.k and not E.selfsync:
                continue
            if k in self.owner:
                assert v <= self.owner[k].n, ("dep on unsignalled", E.name, self.owner[k].name, v)
            if E.waited.get(k, 0) < v:
                sem = self.sems[k]
                E.prog.append(lambda h, sem=sem, v=v: h.wait_ge(sem, v))
                E.waited[k] = v

    @staticmethod
    def _deps(reads, writes):
        deps = {}

        def add(k, v):
            if deps.get(k, 0) < v:
                deps[k] = v
        for b in reads:
            if b.w:
                add(*b.w)
        for b in writes:
            if b.w:
                add(*b.w)
            for k, v in b.r.items():
                add(k, v)
        return deps

    def op(self, E, fn, reads=(), writes=(), sig=True):
        self._waits(E, self._deps(reads, writes))
        val = E.n + 1
        if sig:
            sem = self.sems[E.k]
            E.prog.append(lambda h, fn=fn, sem=sem: fn(h).then_inc(sem, 1))
            E.n = val
        else:
            E.prog.append(lambda h, fn=fn: fn(h))
        for b in reads:
            if b.r.get(E.k, 0) < val:
                b.r[E.k] = val
        for b in writes:
            b.w = (E.k, val)
            b.r = {}

    def dma(self, Q, ob, out_ap, ib, in_ap):
        self._waits(Q, self._deps([ib], [ob]))
        ob.dcnt += 16
        sem = self.sems[ob.dsem]
        Q.prog.append(lambda h, o=out_ap, i=in_ap, sem=sem: h.dma_start(out=o, in_=i).then_inc(sem, 16))
        ob.w = (ob.dsem, ob.dcnt)
        ob.r = {}
        if ib.r.get(ob.dsem, 0) < ob.dcnt:
            ib.r[ob.dsem] = ob.dcnt

    def wait_buf(self, E, b):
        self._waits(E, self._deps([b], []))


def alias_barrier(old, new):
    deps = {}
    for b in old:
        if b.w and deps.get(b.w[0], 0) < b.w[1]:
            deps[b.w[0]] = b.w[1]
        for k, v in b.r.items():
            if deps.get(k, 0) < v:
                deps[k] = v
    for b in new:
        b.w = None
        b.r = dict(deps)


def build(SEQ, DEPTH):
    NT = SEQ // TT
    nc = bass.Bass("TRN2", target_bir_lowering=False)
    cx = Ctx(nc)
    PE = cx.add_engine("pe", False)
    ACT = cx.add_engine("act", SELFSYNC)
    DVE = cx.add_engine("dve", SELFSYNC)
    POOL = cx.add_engine("pool", SELFSYNC)
    SP = Eng("sp", -1, False)

    xin = nc.dram_tensor("xT", [D, SEQ], F32, kind="ExternalInput")
    outT = nc.dram_tensor("outT", [D, SEQ], F32, kind="ExternalOutput")
    consts_d = nc.dram_tensor("consts", [128, NCON], F32, kind="ExternalInput")
    params_d = nc.dram_tensor("params", [DEPTH, 128, NPAR], F32, kind="ExternalInput")
    x1 = nc.dram_tensor("x1T", [D, SEQ], F32)
    xmid_d = nc.dram_tensor("xmidT", [D, TT], F32)
    wspecs = [("win", 84 * 128, 4096), ("pw", 8 * 128, 1024), ("wout", 32 * 128, 4096),
              ("fin", 172 * 128, 4096), ("fout", 4 * 32 * 128, QKC * 128)]
    wsh, wbf, wg, wgbuf = {}, {}, {}, {}
    for l in range(DEPTH):
        for (nm, rows, C) in wspecs:
            key = (l, nm)
            wsh[key] = nc.dram_tensor(f"{nm}{l}", [rows, C], F32, kind="ExternalInput")
            wg[key] = nc.dram_tensor(f"{nm}{l}_g", [rows, C], BF16)
            wgbuf[key] = cx.dbuf(f"wg_{nm}{l}")
    b_x = [Buf("xin"), cx.dbuf("x1"), cx.dbuf("out")]
    b_xmid = cx.dbuf("xmid")
    b_in = Buf("ext")

    import contextlib
    es = contextlib.ExitStack()

    def sb(name, shape, dt):
        return es.enter_context(nc.sbuf_tensor(name, shape, dt))

    def ps(name, shape, dt=F32):
        return es.enter_context(nc.psum_tensor(name, shape, dt))

    with es:
        R1 = sb("R1", [128, 16384], F32)
        R1b = R1[:].bitcast(BF16)
        hT = sb("hT", [128, KC, TT], BF16)
        mixT = sb("mixT", [128, KC, TT], BF16)
        WS = [sb(f"ws{i}", [128, 4096], BF16) for i in range(3)]
        CON = sb("con", [128, 3352], F32)
        PAR = sb("par", [128, NPAR], F32)
        identb = sb("identb", [128, 128], BF16)
        onesb = sb("onesb", [128, 128], BF16)
        ones32 = sb("ones32", [128, 128], F32)
        esink = sb("esink", [128, NH], F32)
        Sst = sb("Sst", [128, NH, 128], F32)
        Sbf = sb("Sbf", [128, NH, 128], BF16)
        kTh = sb("kTh", [128, NKV, 128 + TT], BF16)
        vtm = sb("vtm", [128, NKV, 5, 128], BF16)
        uhalo = sb("uhalo", [128, 2 * NFB, 2], F32)
        XB = [sb(f"xb{i}", [128, TT], F32) for i in range(2)]
        TMP = sb("TMP", [128, 3076], F32)
        f1 = TMP[:, 0:514]
        f2 = TMP[:, 514:1028]
        f3 = TMP[:, 1028:1540]
        f4 = TMP[:, 1540:2052]
        f5 = TMP[:, 2052:2564]
        rstd_t = TMP[:, 2564:3076]
        tA = TMP[:, 0:768].rearrange("p (b x) -> p b x", b=2)
        tC = TMP[:, 768:1152]
        tB = TMP[:, 1152:1536].bitcast(BF16).rearrange("p (b x) -> p b x", b=2)
        r_sq = TMP[:, 1540:1668]
        yhalo = sb("yhalo", [128, 8, 32], F32)
        sqb = [TMP[:, 0:256].bitcast(BF16)]
        sm = TMP[:, 1668:1676]
        _rr = TMP[:, 2052:3076].bitcast(BF16)
        r_kd, r_vt, r_pt, r_qd = (_rr[:, i * 512:(i + 1) * 512] for i in range(4))
        PSB = [ps(f"psb{i}", [128, 512]) for i in range(8)]

        bR1 = cx.dbuf("R1")
        bhT = Buf("hT")
        bmix = Buf("mixT")
        bWS = [cx.dbuf(f"ws{i}") for i in range(3)]
        bCON = cx.dbuf("con")
        bPAR = cx.dbuf("par")
        bmisc = Buf("misc")
        bS = Buf("S")
        bSbf = Buf("Sbf")
        bkTh = Buf("kTh")
        bvtm = Buf("vtm")
        buh = Buf("uhalo")
        bXB = [cx.dbuf(f"xb{i}") for i in range(2)]
        bf = [Buf(f"f{i}") for i in range(7)]
        btA, btB, btC = [bf[1], bf[2]], [bf[3]], [bf[2], bf[3]]
        brstd = Buf("rstd")
        byh = Buf("yhalo")
        bzc = Buf("zc")
        bsq = [bf[1]]
        bsm = bf[4]
        brk = {n: Buf(n) for n in ("kd", "vt", "pt", "qd")}
        brk["sq"] = bf[4]
        bPS = [Buf(f"ps{i}") for i in range(8)]
        ccsem = cx.new_sem("cc")
        cvsem = cx.new_sem("cv")

        state = {"ws": 0, "psg": 0, "psm": 0, "xb": 0, "sq": 0}

        def next_psg():
            i = state["psg"]
            state["psg"] = (i + 1) % 4
            return i

        def next_psm():
            i = 4 + state["psm"]
            state["psm"] = (state["psm"] + 1) % 4
            return i

        c_dist = CON[:, 0:256].rearrange("p (b q) -> p b q", b=2)
        c_dec = CON[:, 256:1792].rearrange("p (h q) -> p h q", h=NH)
        c_qdec = CON[:, 1792:3328].rearrange("p (h q) -> p h q", h=NH)
        c_kdec = CON[:, 3328:3340]
        c_cdec = CON[:, 3340:3352]
        slopes = _slopes(NH)
        p_g = [PAR[:, i * 32:(i + 1) * 32] for i in range(4)]
        p_dww = PAR[:, 128:376].rearrange("p (b j) -> p b j", b=8)
        p_dwb = PAR[:, 376:384]
        p_lng = PAR[:, 384:392]
        p_lnb = PAR[:, 392:400]
        p_fdw = PAR[:, 400:916].rearrange("p (b j) -> p b j", b=2 * NFB)
        p_fdb = PAR[:, 916:1088]
        p_sink = PAR[:, 1088:1100]

        bidn = cx.dbuf("identb")
        cx.dma(POOL, bidn, identb[:], b_in, consts_d[:, 3352:3480])
        for l in range(DEPTH):
            for (nm, rows, C) in wspecs:
                key = (l, nm)
                for r0 in range(0, rows, 128):
                    for c0 in range(0, C, 1024):
                        c1 = min(C, c0 + 1024)
                        cx.dma(POOL, wgbuf[key], wg[key][r0:r0 + 128, c0:c1], b_in, wsh[key][r0:r0 + 128, c0:c1])
                        if wgbuf[key].dcnt % 512 == 0:
                            cx.wait_buf(POOL, wgbuf[key])
        import os
        KD = os.environ.get("KDEBUG", "")

        class _Early(Exception):
            pass

        def stage(name):
            if KD != name:
                return
            for E in (PE, ACT, DVE):
                if E.n:
                    POOL.prog.append(lambda h, sem=cx.sems[E.k], v=E.n: h.wait_ge(sem, v))
            for r0 in range(0, D, 128):
                cx.dma(POOL, b_x[2], outT[r0:r0 + 128, :], b_in, xin[r0:r0 + 128, :])
            raise _Early()

        cx.dma(SP, bCON, CON[:], b_in, consts_d[:, 0:3352])
        cx.op(DVE, lambda h: h.memset(onesb[:], 1.0), [bidn], [bmisc])
        cx.op(DVE, lambda h: h.memset(ones32[:], 1.0), [], [bmisc])

        def load_w(key, row0, ncols):
            i = state["ws"]
            state["ws"] = (i + 1) % 3
            cx.dma(SP, bWS[i], WS[i][:, 0:ncols], wgbuf[key], wg[key][row0:row0 + 128, 0:ncols])
            return i

        def gemm_block(key, row0, kcs, rhs_ap_fn, rhs_buf, bank=None):
            wi = load_w(key, row0, kcs * 128)
            pb = next_psg() if bank is None else bank
            for kc in range(kcs):
                cx.op(PE, lambda h, kc=kc, wi=wi, pb=pb: h.matmul(
                    PSB[pb][:], lhsT=WS[wi][:, kc * 128:(kc + 1) * 128], rhs=rhs_ap_fn(kc),
                    start=(kc == 0), stop=(kc == kcs - 1)),
                    [bWS[wi], rhs_buf], [bPS[pb]], sig=(kc == kcs - 1))
            return pb

        def rstd_from_bank(pb, n):
            cx.op(DVE, lambda h: h.tensor_scalar(out=rstd_t, in0=PSB[pb][:], scalar1=1.0 / n, scalar2=EPS,
                                                 op0=ALU.mult, op1=ALU.add), [bPS[pb]], [brstd])
            cx.op(ACT, lambda h: h.activation(out=rstd_t, in_=rstd_t, func=AF.Sqrt), [brstd], [brstd])
            cx.op(DVE, lambda h: h.reciprocal(out=rstd_t, in_=rstd_t), [brstd], [brstd])

        def sumsq_blocks(src_ap_fn, src_buf, nblk):
            pb = next_psm()
            for kc in range(nblk):
                si = state["sq"]
                state["sq"] = 0
                cx.op(ACT, lambda h, kc=kc, si=si: h.activation(out=sqb[si], in_=src_ap_fn(kc), func=AF.Square),
                      [src_buf], [bsq[si]])
                cx.op(PE, lambda h, kc=kc, si=si, pb=pb: h.matmul(PSB[pb][:], lhsT=onesb[:], rhs=sqb[si],
                                                                 start=(kc == 0), stop=(kc == nblk - 1)),
                      [bsq[si], bmisc], [bPS[pb]], sig=True)
            return pb

        yT = R1[:].rearrange("p (k t) -> p k t", k=KC)

        def main_body():
          for l in range(DEPTH):
              xsrc, bxs = ([xin, x1][l], b_x[l]) if DEPTH == 2 else (xin, b_x[0])
              if l == DEPTH - 1:
                  xdst, bxd = outT, b_x[2]
              else:
                  xdst, bxd = x1, b_x[1]
              xsrc_v = xsrc.ap().rearrange("(k p) s -> p k s", p=128)
              xdst_v = xdst.ap().rearrange("(k p) s -> p k s", p=128)
              xmid_v = xmid_d.ap().rearrange("(k p) s -> p k s", p=128)
              cx.dma(SP, bPAR, PAR[:], b_in, params_d[l, :, :])
              cx.op(ACT, lambda h: h.activation(out=esink[:], in_=p_sink, func=AF.Exp), [bPAR], [bmisc])
              cx.op(DVE, lambda h: h.memset(Sst[:], 0.0), [], [bS])
              cx.op(DVE, lambda h: h.memset(Sbf[:], 0.0), [], [bSbf])
              cx.op(DVE, lambda h: h.memset(uhalo[:], 0.0), [], [buh])
              cx.op(DVE, lambda h: h.memset(kTh[:, :, 0:128], 0.0), [], [bkTh])
              cx.op(DVE, lambda h: h.memset(vtm[:, :, 0, :], 0.0), [], [bvtm])

              for t in range(NT):
                  tok = slice(t * TT, (t + 1) * TT)
                  for k0 in range(0, KC, 4):
                      cx.dma(SP, bR1, yT[:, k0:k0 + 4, :], bxs, xsrc_v[:, k0:k0 + 4, tok])
                  pb = sumsq_blocks(lambda kc: yT[:, kc, :], bR1, KC)
                  rstd_from_bank(pb, D)
                  for kc in range(KC):
                      cx.op(DVE, lambda h, kc=kc: h.scalar_tensor_tensor(
                          out=hT[:, kc, :], in0=yT[:, kc, :], scalar=p_g[0][:, kc:kc + 1], in1=rstd_t,
                          op0=ALU.mult, op1=ALU.mult), [bR1, bPAR, brstd], [bhT])
                  hrhs = lambda kc: hT[:, kc, :]
                  stage("n1")

                  def slot(i):
                      return R1b[:, i * TT:(i + 1) * TT]
                  for blk in range(20):
                      pb = gemm_block((l, "win"), blk * 128, KC, hrhs, bhT)
                      if 12 <= blk < 16:
                          dst = kTh[:, blk - 12, 128:128 + TT]
                          cx.op(ACT, lambda h, pb=pb, dst=dst: h.activation(out=dst, in_=PSB[pb][:], func=AF.Copy),
                                [bPS[pb]], [bkTh])
                      else:
                          cx.op(ACT, lambda h, pb=pb, blk=blk: h.activation(out=slot(blk), in_=PSB[pb][:], func=AF.Copy),
                                [bPS[pb]], [bR1])
                  stage("attg")
                  for g in range(NKV):
                      pb = next_psm()
                      pbv = PSB[pb][:].bitcast(BF16)
                      for c in range(4):
                          cx.op(PE, lambda h, g=g, c=c, pbv=pbv: h.transpose(
                              pbv[:, c * 128:(c + 1) * 128], slot(16 + g)[:, c * 128:(c + 1) * 128], identb[:]),
                              [bR1, bmisc], [bPS[pb]], sig=(c == 3))
                      cx.op(ACT, lambda h, g=g, pbv=pbv: h.activation(
                          out=vtm[:, g, 1:5, :], in_=pbv[:, 0:512].rearrange("p (c d) -> p c d", c=4), func=AF.Copy),
                          [bPS[pb]], [bvtm])
                  stage("attv")
                  scale = HD ** -0.5
                  for g in range(NKV):
                      for c in range(4):
                          first = (t == 0 and c == 0)
                          qv = R1b[:, 0:NH * TT].rearrange("p (h t) -> p h t", h=NH)[:, 3 * g:3 * g + 3, c * 128:(c + 1) * 128]
                          blocks = [1] if first else [0, 1]
                          pbs = {}
                          for bi in blocks:
                              pb = next_psm()
                              pbs[bi] = pb
                              kap = kTh[:, g, c * 128 + bi * 128: c * 128 + bi * 128 + 128]
                              cx.op(PE, lambda h, pb=pb, kap=kap, qv=qv: h.matmul(
                                  PSB[pb][:, 0:384].rearrange("p (h q) -> p h q", h=3), lhsT=kap, rhs=qv,
                                  start=True, stop=True), [bkTh, bR1], [bPS[pb]])
                          for bi in blocks:
                              pb = pbs[bi]
                              for hh in range(3):
                                  cx.op(DVE, lambda h, pb=pb, bi=bi, g=g, hh=hh: h.scalar_tensor_tensor(
                                      out=tA[:, bi, hh * 128:(hh + 1) * 128], in0=c_dist[:, bi, :],
                                      scalar=-slopes[3 * g + hh] / scale,
                                      in1=PSB[pb][:, hh * 128:(hh + 1) * 128], op0=ALU.mult, op1=ALU.add),
                                      [bPS[pb], bCON], btA)
                          lo = blocks[0]
                          cx.op(ACT, lambda h, lo=lo: h.activation(out=tB[:, lo:2, :], in_=tA[:, lo:2, :], func=AF.Exp, scale=scale),
                                btA, btB)
                          po = next_psm()
                          for j, bi in enumerate(blocks):
                              cx.op(PE, lambda h, po=po, bi=bi, g=g, c=c, j=j, nb=len(blocks): h.matmul(
                                  PSB[po][:, 0:384], lhsT=vtm[:, g, c + bi, :], rhs=tB[:, bi, :],
                                  start=(j == 0), stop=(j == nb - 1)),
                                  [bvtm] + btB, [bPS[po]], sig=(j == len(blocks) - 1))
                          pd = next_psm()
                          for j, bi in enumerate(blocks):
                              cx.op(PE, lambda h, pd=pd, bi=bi, j=j, nb=len(blocks): h.matmul(
                                  PSB[pd][:, 0:384], lhsT=onesb[:], rhs=tB[:, bi, :],
                                  start=(j == 0), stop=(j == nb - 1)),
                                  [bmisc] + btB, [bPS[pd]], sig=(j == len(blocks) - 1))
                          for hh in range(3):
                              cx.op(DVE, lambda h, pd=pd, hh=hh, g=g: h.tensor_scalar(
                                  out=tC[:, hh * 128:(hh + 1) * 128], in0=PSB[pd][:, hh * 128:(hh + 1) * 128],
                                  scalar1=esink[:, 3 * g + hh:3 * g + hh + 1], scalar2=None, op0=ALU.add),
                                  [bPS[pd], bmisc], btC)
                          cx.op(DVE, lambda h: h.reciprocal(out=tC, in_=tC), btC, btC)
                          cx.op(DVE, lambda h, po=po, g=g, c=c: h.tensor_tensor(
                              out=mixT[:, 3 * g:3 * g + 3, c * 128:(c + 1) * 128],
                              in0=PSB[po][:, 0:384].rearrange("p (h q) -> p h q", h=3),
                              in1=tC.rearrange("p (h q) -> p h q", h=3), op=ALU.mult),
                              [bPS[po]] + btC, [bmix])
                  cx.op(DVE, lambda h: h.tensor_copy(out=kTh[:, :, 0:128], in_=kTh[:, :, TT:TT + 128]), [bkTh], [bkTh])
                  cx.op(DVE, lambda h: h.tensor_copy(out=vtm[:, :, 0, :], in_=vtm[:, :, 4, :]), [bvtm], [bvtm])

                  stage("att")
                  yTc = R1[:, 0:8 * 544].rearrange("p (b t) -> p b t", b=8)
                  sTc = R1b[:, 9216:9216 + 8 * TT].rearrange("p (b t) -> p b t", b=8)
                  zc = R1[:, 8192:8192 + 8 * TT].rearrange("p (b t) -> p b t", b=8)
                  alias_barrier([bR1], [bzc])
                  if t == 0:
                      cx.op(DVE, lambda h: h.memset(yTc[:, :, 0:32], 0.0), [], [bR1])
                  else:
                      cx.op(DVE, lambda h: h.tensor_copy(out=yTc[:, :, 0:32], in_=yhalo[:]), [byh], [bR1])
                  ps1 = next_psm()
                  ps2 = next_psm()
                  deferred = []
                  for i in range(8):
                      pa = gemm_block((l, "win"), (20 + i) * 128, KC, hrhs, bhT)
                      pg = gemm_block((l, "win"), (28 + i) * 128, KC, hrhs, bhT)
                      for d_ in deferred:
                          d_()
                      deferred = []
                      cx.op(ACT, lambda h, pg=pg: h.activation(out=f3, in_=PSB[pg][:], func=AF.Sigmoid), [bPS[pg]], [bf[3]])
                      cx.op(DVE, lambda h, pa=pa, i=i: h.tensor_tensor(out=yTc[:, i, 32:544], in0=PSB[pa][:], in1=f3, op=ALU.mult),
                            [bPS[pa], bf[3]], [bR1])
                      cx.op(ACT, lambda h, i=i: h.activation(out=zc[:, i, :], in_=yTc[:, i, 2:2 + TT], func=AF.Identity,
                                                             bias=p_dwb[:, i:i + 1], scale=p_dww[:, i, 0:1]), [bR1, bPAR], [bzc])
                      for j in range(1, CW):
                          cx.op(DVE, lambda h, i=i, j=j: h.scalar_tensor_tensor(out=zc[:, i, :], in0=yTc[:, i, 2 + j:2 + j + TT], scalar=p_dww[:, i, j:j + 1],
                                                                               in1=zc[:, i, :], op0=ALU.mult, op1=ALU.add), [bR1, bPAR, bzc], [bzc])
                      cx.op(ACT, lambda h, i=i: h.activation(out=f4, in_=zc[:, i, :], func=AF.Square), [bzc], [bf[4]])
                      deferred.append(lambda i=i, ps1=ps1: cx.op(PE, lambda h: h.matmul(PSB[ps1][:], lhsT=ones32[:], rhs=zc[:, i, :], start=(i == 0), stop=(i == 7)),
                                                                 [bzc, bmisc], [bPS[ps1]]))
                      deferred.append(lambda i=i, ps2=ps2: cx.op(PE, lambda h: h.matmul(PSB[ps2][:], lhsT=ones32[:], rhs=f4, start=(i == 0), stop=(i == 7)),
                                                                 [bf[4], bmisc], [bPS[ps2]]))
                  for d_ in deferred:
                      d_()
                  cx.op(DVE, lambda h: h.tensor_copy(out=yhalo[:], in_=yTc[:, :, 512:544]), [bR1], [byh])
                  cx.op(DVE, lambda h, ps1=ps1: h.tensor_scalar(out=f4, in0=PSB[ps1][:], scalar1=1.0 / CONVC, scalar2=None, op0=ALU.mult), [bPS[ps1]], [bf[4]])
                  cx.op(DVE, lambda h: h.tensor_tensor(out=f5, in0=f4, in1=f4, op=ALU.mult), [bf[4]], [bf[5]])
                  cx.op(DVE, lambda h, ps2=ps2: h.scalar_tensor_tensor(out=f5, in0=PSB[ps2][:], scalar=1.0 / CONVC, in1=f5, op0=ALU.mult, op1=ALU.subtract),
                        [bPS[ps2], bf[5]], [bf[5]])
                  cx.op(DVE, lambda h: h.tensor_scalar(out=f5, in0=f5, scalar1=EPS, scalar2=None, op0=ALU.add), [bf[5]], [bf[5]])
                  cx.op(ACT, lambda h: h.activation(out=f5, in_=f5, func=AF.Sqrt), [bf[5]], [bf[5]])
                  cx.op(DVE, lambda h: h.reciprocal(out=f5, in_=f5), [bf[5]], [bf[5]])
                  for i in range(8):
                      cx.op(DVE, lambda h, i=i: h.tensor_tensor(out=f3, in0=zc[:, i, :], in1=f4, op=ALU.subtract), [bzc, bf[4]], [bf[3]])
                      cx.op(DVE, lambda h: h.tensor_tensor(out=f3, in0=f3, in1=f5, op=ALU.mult), [bf[3], bf[5]], [bf[3]])
                      cx.op(ACT, lambda h, i=i: h.activation(out=sTc[:, i, :], in_=f3, func=AF.Silu, bias=p_lnb[:, i:i + 1], scale=p_lng[:, i:i + 1]),
                            [bf[3], bPAR], [bR1])
                  for ob in range(8):
                      pb = gemm_block((l, "pw"), ob * 128, 8, lambda kc: sTc[:, kc, :], bR1)
                      cx.op(ACT, lambda h, pb=pb, ob=ob: h.activation(out=mixT[:, 12 + ob, :], in_=PSB[pb][:], func=AF.Copy), [bPS[pb]], [bmix])

                  alias_barrier([bR1, bzc], [bR1])
                  stage("conv")
                  rbufs = [brk[n] for n in ("kd", "vt", "pt", "qd")]
                  alias_barrier([bf[5], brstd], rbufs)
                  bH = [Buf("H0"), Buf("H1")]
                  alias_barrier([bR1], bH)

                  def ret_gemm(half):
                      for hh in range(6):
                          hd = half * 6 + hh
                          for kind, base in enumerate((36, 48, 60, 72)):
                              pb = gemm_block((l, "win"), (base + hd) * 128, KC, hrhs, bhT)
                              fn = AF.Silu if kind == 3 else AF.Copy
                              cx.op(ACT, lambda h, pb=pb, s=24 * half + kind * 6 + hh, fn=fn: h.activation(out=slot(s), in_=PSB[pb][:], func=fn),
                                    [bPS[pb]], [bH[half]])
                              yield

                  def ret_chain(half):
                      bRh = bH[half]
                      for hh in range(6):
                          hd = half * 6 + hh
                          qs, ks, vs, gs = (slot(24 * half + kk * 6 + hh) for kk in range(4))
                          S4 = Sbf[:, 4 * (hd % 3):4 * (hd % 3) + 4, :]
                          pk_ = next_psm()
                          pkv = PSB[pk_][:].bitcast(BF16)
                          for c in range(4):
                              cx.op(PE, lambda h, pkv=pkv, ks=ks, c=c: h.transpose(pkv[:, c * 128:(c + 1) * 128], ks[:, c * 128:(c + 1) * 128], identb[:]),
                                    [bRh, bmisc], [bPS[pk_]], sig=(c == 3))
                          cx.op(DVE, lambda h, pkv=pkv, hd=hd: h.tensor_scalar(out=r_kd, in0=pkv[:, 0:512], scalar1=c_kdec[:, hd:hd + 1], scalar2=None, op0=ALU.mult),
                                [bPS[pk_], bCON], [brk["kd"]])
                          pv_ = next_psm()
                          pvv = PSB[pv_][:].bitcast(BF16)
                          for c in range(4):
                              cx.op(PE, lambda h, pvv=pvv, vs=vs, c=c: h.transpose(pvv[:, c * 128:(c + 1) * 128], vs[:, c * 128:(c + 1) * 128], identb[:]),
                                    [bRh, bmisc], [bPS[pv_]], sig=(c == 3))
                          cx.op(ACT, lambda h, pvv=pvv: h.activation(out=r_vt, in_=pvv[:, 0:512], func=AF.Copy), [bPS[pv_]], [brk["vt"]])
                          yield
                          pkv_ = next_psm()
                          for c in range(4):
                              cx.op(PE, lambda h, pkv_=pkv_, c=c: h.matmul(PSB[pkv_][:, c * 128:(c + 1) * 128], lhsT=r_kd[:, c * 128:(c + 1) * 128],
                                                                           rhs=r_vt[:, c * 128:(c + 1) * 128], start=True, stop=True),
                                    [brk["kd"], brk["vt"]], [bPS[pkv_]], sig=(c == 3))
                          for c in range(4):
                              cx.op(ACT, lambda h, hd=hd, c=c, S4=S4: h.activation(out=S4[:, c, :], in_=Sst[:, hd, :], func=AF.Copy), [bS], [bSbf])
                              cx.op(DVE, lambda h, pkv_=pkv_, hd=hd, c=c: h.scalar_tensor_tensor(out=Sst[:, hd, :], in0=Sst[:, hd, :], scalar=c_cdec[:, hd:hd + 1],
                                                                                            in1=PSB[pkv_][:, c * 128:(c + 1) * 128], op0=ALU.mult, op1=ALU.add),
                                    [bS, bCON, bPS[pkv_]], [bS])
                          pss = next_psm()
                          for c in range(4):
                              cx.op(PE, lambda h, pss=pss, ks=ks, qs=qs, c=c: h.matmul(PSB[pss][:, c * 128:(c + 1) * 128], lhsT=ks[:, c * 128:(c + 1) * 128],
                                                                                       rhs=qs[:, c * 128:(c + 1) * 128], start=True, stop=True),
                                    [bRh], [bPS[pss]], sig=(c == 3))
                          for c in range(4):
                              cx.op(DVE, lambda h, pss=pss, hd=hd, c=c: h.tensor_tensor(out=r_pt[:, c * 128:(c + 1) * 128], in0=PSB[pss][:, c * 128:(c + 1) * 128],
                                                                                        in1=c_dec[:, hd, :], op=ALU.mult), [bPS[pss], bCON], [brk["pt"]])
                              cx.op(DVE, lambda h, qs=qs, hd=hd, c=c: h.tensor_tensor(out=r_qd[:, c * 128:(c + 1) * 128], in0=qs[:, c * 128:(c + 1) * 128],
                                                                                      in1=c_qdec[:, hd, :], op=ALU.mult), [bRh, bCON], [brk["qd"]])
                          yield
                          po = next_psm()
                          for c in range(4):
                              cx.op(PE, lambda h, po=po, c=c: h.matmul(PSB[po][:, c * 128:(c + 1) * 128], lhsT=r_vt[:, c * 128:(c + 1) * 128],
                                                                       rhs=r_pt[:, c * 128:(c + 1) * 128], start=True, stop=False),
                                    [brk["vt"], brk["pt"]], [bPS[po]], sig=False)
                              cx.op(PE, lambda h, po=po, c=c, S4=S4: h.matmul(PSB[po][:, c * 128:(c + 1) * 128], lhsT=S4[:, c, :],
                                                                              rhs=r_qd[:, c * 128:(c + 1) * 128], start=False, stop=True),
                                    [bSbf, brk["qd"]], [bPS[po]], sig=(c == 3))
                          cx.op(ACT, lambda h, po=po: h.activation(out=f1[:, 0:TT], in_=PSB[po][:], func=AF.Copy), [bPS[po]], [bf[1]])
                          cx.op(ACT, lambda h, po=po: h.activation(out=f2[:, 0:TT], in_=PSB[po][:], func=AF.Square), [bPS[po]], [bf[2]])
                          yield
                          pm = next_psm()
                          cx.op(PE, lambda h, pm=pm: h.matmul(PSB[pm][:], lhsT=ones32[:], rhs=f1[:, 0:TT], start=True, stop=True), [bf[1], bmisc], [bPS[pm]])
                          pq2 = next_psm()
                          cx.op(PE, lambda h, pq2=pq2: h.matmul(PSB[pq2][:], lhsT=ones32[:], rhs=f2[:, 0:TT], start=True, stop=True), [bf[2], bmisc], [bPS[pq2]])
                          cx.op(DVE, lambda h, pm=pm: h.tensor_scalar(out=f3, in0=PSB[pm][:], scalar1=1.0 / HD, scalar2=None, op0=ALU.mult), [bPS[pm]], [bf[3]])
                          cx.op(DVE, lambda h: h.tensor_tensor(out=f4, in0=f3, in1=f3, op=ALU.mult), [bf[3]], [bf[4]])
                          cx.op(DVE, lambda h, pq2=pq2: h.scalar_tensor_tensor(out=f4, in0=PSB[pq2][:], scalar=1.0 / HD, in1=f4, op0=ALU.mult, op1=ALU.subtract),
                                [bPS[pq2], bf[4]], [bf[4]])
                          cx.op(DVE, lambda h: h.tensor_scalar(out=f4, in0=f4, scalar1=EPS, scalar2=None, op0=ALU.add), [bf[4]], [bf[4]])
                          cx.op(ACT, lambda h: h.activation(out=f4, in_=f4, func=AF.Sqrt), [bf[4]], [bf[4]])
                          cx.op(DVE, lambda h: h.reciprocal(out=f4, in_=f4), [bf[4]], [bf[4]])
                          cx.op(DVE, lambda h: h.tensor_tensor(out=f1[:, 0:TT], in0=f1[:, 0:TT], in1=f3, op=ALU.subtract), [bf[1], bf[3]], [bf[1]])
                          cx.op(DVE, lambda h: h.tensor_tensor(out=f1[:, 0:TT], in0=f1[:, 0:TT], in1=f4, op=ALU.mult), [bf[1], bf[4]], [bf[1]])
                          cx.op(DVE, lambda h, gs=gs, hd=hd: h.tensor_tensor(out=mixT[:, 20 + hd, :], in0=f1[:, 0:TT], in1=gs, op=ALU.mult), [bf[1], bRh], [bmix])
                          yield

                  for _ in ret_gemm(0):
                      pass
                  ch0 = ret_chain(0)
                  for _ in ret_gemm(1):
                      next(ch0, None)
                  for _ in ch0:
                      pass
                  for _ in ret_chain(1):
                      pass
                  alias_barrier(bH, [bR1])
                  alias_barrier(rbufs, [bf[5], brstd])
                  stage("ret")
                  pstat = next_psm()
                  wdef = []
                  for ob in range(KC):
                      pb = gemm_block((l, "wout"), ob * 128, KC, lambda kc: mixT[:, kc, :], bmix)
                      cx.op(ACT, lambda h, pb=pb, ob=ob: h.activation(out=yT[:, ob, :], in_=PSB[pb][:], func=AF.Copy), [bPS[pb]], [bR1])
                      si = state["sq"]
                      state["sq"] = 0
                      for d_ in wdef:
                          d_()
                      wdef = []
                      cx.op(ACT, lambda h, pb=pb, si=si: h.activation(out=sqb[si], in_=PSB[pb][:], func=AF.Square), [bPS[pb]], [bsq[si]])
                      wdef.append(lambda si=si, ob=ob, pstat=pstat: cx.op(PE, lambda h: h.matmul(PSB[pstat][:], lhsT=onesb[:], rhs=sqb[si], start=(ob == 0), stop=(ob == KC - 1)),
                                                                          [bsq[si], bmisc], [bPS[pstat]]))
                  for d_ in wdef:
                      d_()
                  rstd_from_bank(pstat, D)
                  stage("wout")
                  pstat2 = next_psm()
                  for ob in range(KC):
                      xi = state["xb"]
                      state["xb"] = (xi + 1) % 2
                      cx.dma(SP, bXB[xi], XB[xi][:], bxs, xsrc_v[:, ob, tok])
                      cx.op(DVE, lambda h, ob=ob: h.tensor_tensor(out=yT[:, ob, :], in0=yT[:, ob, :], in1=rstd_t, op=ALU.mult), [bR1, brstd], [bR1])
                      cx.op(DVE, lambda h, ob=ob, xi=xi: h.scalar_tensor_tensor(out=yT[:, ob, :], in0=yT[:, ob, :], scalar=p_g[1][:, ob:ob + 1], in1=XB[xi][:],
                                                                               op0=ALU.mult, op1=ALU.add), [bR1, bPAR, bXB[xi]], [bR1])
                      si = state["sq"]
                      state["sq"] = 0
                      cx.op(ACT, lambda h, ob=ob, si=si: h.activation(out=sqb[si], in_=yT[:, ob, :], func=AF.Square), [bR1], [bsq[si]])
                      cx.op(PE, lambda h, si=si, ob=ob, pstat2=pstat2: h.matmul(PSB[pstat2][:], lhsT=onesb[:], rhs=sqb[si], start=(ob == 0), stop=(ob == KC - 1)),
                            [bsq[si], bmisc], [bPS[pstat2]])
                  for k0 in range(0, KC, 4):
                      cx.dma(SP, b_xmid, xmid_v[:, k0:k0 + 4, :], bR1, yT[:, k0:k0 + 4, :])
                  rstd_from_bank(pstat2, D)
                  for kc in range(KC):
                      cx.op(DVE, lambda h, kc=kc: h.scalar_tensor_tensor(out=hT[:, kc, :], in0=yT[:, kc, :], scalar=p_g[2][:, kc:kc + 1], in1=rstd_t,
                                                                        op0=ALU.mult, op1=ALU.mult), [bR1, bPAR, brstd], [bhT])

                  stage("res1")
                  qoff = 0
                  for q in range(4):
                      nq = QSZ[q]
                      for ii in range(nq):
                          i = qoff + ii
                          pg = gemm_block((l, "fin"), i * 128, KC, hrhs, bhT)
                          pv = gemm_block((l, "fin"), (NFB + i) * 128, KC, hrhs, bhT)
                          res = {}
                          for (nm, pbk, bi, fb, bfb) in (("g", pg, i, f1, bf[1]), ("v", pv, NFB + i, f2, bf[2])):
                              cx.op(ACT, lambda h, pbk=pbk, fb=fb: h.activation(out=fb[:, 2:TT + 2], in_=PSB[pbk][:], func=AF.Copy), [bPS[pbk]], [bfb])
                              cx.op(ACT, lambda h, fb=fb, bi=bi: h.activation(out=fb[:, 0:2], in_=uhalo[:, bi, :], func=AF.Copy), [buh], [bfb])
                              cx.op(ACT, lambda h, fb=fb, bi=bi: h.activation(out=uhalo[:, bi, :], in_=fb[:, TT:TT + 2], func=AF.Copy), [bfb], [buh])
                              dst, bdst = (f3, bf[3]) if nm == "g" else (f4, bf[4])
                              cx.op(ACT, lambda h, fb=fb, bi=bi, dst=dst: h.activation(out=dst[:], in_=fb[:, 0:TT], func=AF.Identity,
                                                                                      bias=p_fdb[:, bi:bi + 1], scale=p_fdw[:, bi, 0:1]), [bfb, bPAR], [bdst])
                              cx.op(DVE, lambda h, fb=fb, bi=bi, dst=dst: h.scalar_tensor_tensor(out=dst[:], in0=fb[:, 1:TT + 1], scalar=p_fdw[:, bi, 1:2], in1=dst[:],
                                                                                                op0=ALU.mult, op1=ALU.add), [bfb, bPAR, bdst], [bdst])
                              cx.op(DVE, lambda h, fb=fb, bi=bi, dst=dst: h.scalar_tensor_tensor(out=dst[:], in0=fb[:, 2:TT + 2], scalar=p_fdw[:, bi, 2:3], in1=dst[:],
                                                                                                op0=ALU.mult, op1=ALU.add), [bfb, bPAR, bdst], [bdst])
                          cx.op(DVE, lambda h: h.tensor_tensor(out=f5, in0=f3, in1=f3, op=ALU.mult), [bf[3]], [bf[5]])
                          cx.op(DVE, lambda h: h.tensor_scalar(out=f5, in0=f5, scalar1=0.044715, scalar2=1.0, op0=ALU.mult, op1=ALU.add), [bf[5]], [bf[5]])
                          cx.op(DVE, lambda h: h.tensor_tensor(out=f5, in0=f5, in1=f3, op=ALU.mult), [bf[5], bf[3]], [bf[5]])
                          cx.op(ACT, lambda h: h.activation(out=f5, in_=f5, func=AF.Sigmoid, scale=1.5957691216057308), [bf[5]], [bf[5]])
                          cx.op(DVE, lambda h: h.tensor_tensor(out=f5, in0=f5, in1=f3, op=ALU.mult), [bf[5], bf[3]], [bf[5]])
                          cx.op(DVE, lambda h, ii=ii: h.tensor_tensor(out=mixT[:, ii, :], in0=f5, in1=f4, op=ALU.mult), [bf[5], bf[4]], [bmix])
                      for ob in range(KC):
                          pb = gemm_block((l, "fout"), (q * KC + ob) * 128, nq, lambda kc: mixT[:, kc, :], bmix)
                          if q == 0:
                              cx.op(ACT, lambda h, pb=pb, ob=ob: h.activation(out=yT[:, ob, :], in_=PSB[pb][:], func=AF.Copy), [bPS[pb]], [bR1])
                          else:
                              cx.op(DVE, lambda h, pb=pb, ob=ob: h.tensor_tensor(out=yT[:, ob, :], in0=yT[:, ob, :], in1=PSB[pb][:], op=ALU.add), [bR1, bPS[pb]], [bR1])
                      qoff += nq
                  stage("ffn")
                  pb = sumsq_blocks(lambda kc: yT[:, kc, :], bR1, KC)
                  rstd_from_bank(pb, D)
                  for ob in range(KC):
                      xi = state["xb"]
                      state["xb"] = (xi + 1) % 2
                      cx.dma(SP, bXB[xi], XB[xi][:], b_xmid, xmid_v[:, ob, :])
                      cx.op(DVE, lambda h, ob=ob: h.tensor_tensor(out=yT[:, ob, :], in0=yT[:, ob, :], in1=rstd_t, op=ALU.mult), [bR1, brstd], [bR1])
                      cx.op(DVE, lambda h, ob=ob, xi=xi: h.scalar_tensor_tensor(out=yT[:, ob, :], in0=yT[:, ob, :], scalar=p_g[3][:, ob:ob + 1], in1=XB[xi][:],
                                                                               op0=ALU.mult, op1=ALU.add), [bR1, bPAR, bXB[xi]], [bR1])
                  for k0 in range(0, KC, 4):
                      cx.dma(SP, bxd, xdst_v[:, k0:k0 + 4, tok], bR1, yT[:, k0:k0 + 4, :])
        try:
            main_body()
        except _Early:
            pass
        cx.wait_buf(POOL, b_x[2])

        with nc.Block() as block:
            @block.tensor
            def _(h):
                for f in PE.prog:
                    f(h)

            @block.scalar
            def _(h):
                for f in ACT.prog:
                    f(h)

            @block.vector
            def _(h):
                for f in DVE.prog:
                    f(h)

            @block.gpsimd
            def _(h):
                for f in POOL.prog:
                    f(h)

            @block.sync
            def _(h):
                for f in SP.prog:
                    f(h)
    return nc


def _tile_w(W):
    K, N = W.shape
    return np.ascontiguousarray(W.reshape(K // 128, 128, N // 128, 128).transpose(2, 1, 0, 3)).reshape(N // 128 * 128, K)


def _consts():
    c = np.zeros((128, NCON), np.float32)
    j = np.arange(128)[:, None].astype(np.float64)
    i = np.arange(128)[None, :].astype(np.float64)
    dp = i + 128 - j
    dc = i - j
    c[:, 0:128] = np.where(dp < 128, dp, 1e9)
    c[:, 128:256] = np.where(dc >= 0, dc, 1e9)
    lg = np.log1p(-np.exp2(-5.0 - np.arange(NH, dtype=np.float64)))
    dec = np.zeros((128, NH, 128), np.float64)
    qdec = np.zeros((128, NH, 128), np.float64)
    for h in range(NH):
        rel = i - j
        dec[:, h, :] = np.where(rel >= 0, np.exp(lg[h] * np.maximum(rel, 0.0)), 0.0) * (HD ** -0.5)
        qdec[:, h, :] = np.exp(lg[h] * (i + 1.0))
    c[:, 256:1792] = dec.reshape(128, -1)
    c[:, 1792:3328] = qdec.reshape(128, -1)
    c[:, 3328:3340] = np.exp(lg[None, :] * (127.0 - j)) * (HD ** -0.5)
    c[:, 3340:3352] = np.exp(lg * 128.0)[None, :]
    c[:, 3352:3480] = np.eye(128, dtype=np.float32)
    return c


def _fm(v, nb):
    return np.ascontiguousarray(np.asarray(v).reshape(nb, 128).T)


_CACHE = {}


def kernel(x, mix_pre_g, mix_post_g, ffn_pre_g, ffn_post_g, w_in, att_sinks, conv_dw_w, conv_dw_b,
           conv_ln_g, conv_ln_b, conv_pw_w, w_out, ffn_w_in, ffn_dw_w, ffn_dw_b, ffn_w_out):
    x = np.asarray(x, np.float32)
    B, SEQ, _ = x.shape
    DEPTH = np.asarray(w_in).shape[0]
    kk = (SEQ, DEPTH)
    if kk not in _CACHE:
        _CACHE[kk] = build(SEQ, DEPTH)
    nc = _CACHE[kk]
    consts = _consts()
    params = np.zeros((DEPTH, 128, NPAR), np.float32)
    shards = [dict() for _ in range(NCORE)]
    for l in range(DEPTH):
        p = params[l]
        p[:, 0:32] = _fm(mix_pre_g[l], 32)
        p[:, 32:64] = _fm(mix_post_g[l], 32)
        p[:, 64:96] = _fm(ffn_pre_g[l], 32)
        p[:, 96:128] = _fm(ffn_post_g[l], 32)
        dww = np.asarray(conv_dw_w[l])
        p[:, 128:376] = dww.reshape(CW, 8, 128).transpose(2, 1, 0).reshape(128, -1)
        p[:, 376:384] = _fm(conv_dw_b[l], 8)
        p[:, 384:392] = _fm(conv_ln_g[l], 8)
        p[:, 392:400] = _fm(conv_ln_b[l], 8)
        fdw = np.asarray(ffn_dw_w[l])
        p[:, 400:916] = fdw.reshape(3, 2 * NFB, 128).transpose(2, 1, 0).reshape(128, -1)
        p[:, 916:1088] = _fm(ffn_dw_b[l], 2 * NFB)
        p[:, 1088:1100] = np.asarray(att_sinks[l])[None, :]
        fo = np.asarray(ffn_w_out[l], np.float32)
        fparts = []
        r0 = 0
        for q in range(4):
            kq = QSZ[q] * 128
            tq = _tile_w(fo[r0:r0 + kq])
            if kq < QKC * 128:
                tq = np.concatenate([tq, np.zeros((tq.shape[0], QKC * 128 - kq), np.float32)], axis=1)
            fparts.append(tq)
            r0 += kq
        tiled = {"win": _tile_w(np.asarray(w_in[l], np.float32)), "pw": _tile_w(np.asarray(conv_pw_w[l], np.float32)),
                 "wout": _tile_w(np.asarray(w_out[l], np.float32)), "fin": _tile_w(np.asarray(ffn_w_in[l], np.float32)),
                 "fout": np.concatenate(fparts, axis=0)}
        for nm, arr in tiled.items():
            for c in range(NCORE):
                shards[c][f"{nm}{l}"] = arr
        del tiled, fparts, fo
    if B == 4:
        ncore, active = 8, [0, 1, 4, 5]
    else:
        ncore, active = B, list(range(B))
    in_maps = [None] * ncore
    zeros = {k: np.zeros_like(v) for k, v in shards[0].items()}
    zx = np.zeros((D, SEQ), np.float32)
    for c in range(ncore):
        if c in active:
            m = dict(shards[0])
            m["xT"] = np.ascontiguousarray(x[active.index(c)].T)
        else:
            m = dict(zeros)
            m["xT"] = zx
        m["consts"] = consts
        m["params"] = params
        in_maps[c] = m
    res = run_bass_kernel_spmd(nc, in_maps, core_ids=list(range(ncore)))
    out = np.stack([np.ascontiguousarray(res.results[active[b]]["outT"].T) for b in range(B)], axis=0)
    return out.astype(np.float32)
```

```python
import math
import numpy as np
import concourse.bass as bass
import concourse.mybir as mybir
from concourse.bass_utils import run_bass_kernel_spmd

F32 = mybir.dt.float32
BF16 = mybir.dt.bfloat16
AF = mybir.ActivationFunctionType
ALU = mybir.AluOpType
AX = mybir.AxisListType

D = 4096
KC = 32
NCORE = 8
TT = 512
HD = 128
NH = 12
NKV = 4
CONVC = 1024
CW = 31
DFF = 11008
NFB = 86
QSZ = [22, 22, 21, 21]
QKC = 22
EPS = 1e-6
NEG = -30000.0
NPAR = 1100
NCON = 3480
SELFSYNC = True


def _slopes(n):
    def p2(n):
        s = 2.0 ** (-8.0 / n)
        return [s ** (i + 1) for i in range(n)]
    if math.log2(n).is_integer():
        return p2(n)
    c = 2 ** math.floor(math.log2(n))
    return p2(c) + _slopes(2 * c)[0::2][: n - c]


class Buf:
    def __init__(self, name, dsem=None):
        self.name = name
        self.w = None
        self.r = {}
        self.dsem = dsem
        self.dcnt = 0


class Eng:
    def __init__(self, name, k, selfsync):
        self.name = name
        self.k = k
        self.n = 0
        self.waited = {}
        self.selfsync = selfsync
        self.prog = []


class Ctx:
    def __init__(self, nc):
        self.nc = nc
        self.sems = []
        self.owner = {}
        self.engs = {}

    def new_sem(self, name):
        s = self.nc.alloc_semaphore(name)
        self.sems.append(s)
        return len(self.sems) - 1

    def add_engine(self, name, selfsync):
        k = self.new_sem("p_" + name)
        e = Eng(name, k, selfsync)
        self.owner[k] = e
        self.engs[name] = e
        return e

    def dbuf(self, name):
        return Buf(name, self.new_sem("d_" + name))

    def _waits(self, E, deps):
        for k, v in deps.items():
            if k == E.k and not E.selfsync:
                continue
            if k in self.owner:
                assert v <= self.owner[k].n, ("dep on unsignalled", E.name, self.owner[k].name, v)
            if E.waited.get(k, 0) < v:
                sem = self.sems[k]
                E.prog.append(lambda h, sem=sem, v=v: h.wait_ge(sem, v))
                E.waited[k] = v

    @staticmethod
    def _deps(reads, writes):
        deps = {}

        def add(k, v):
            if deps.get(k, 0) < v:
                deps[k] = v
        for b in reads:
            if b.w:
                add(*b.w)
        for b in writes:
            if b.w:
                add(*b.w)
            for k, v in b.r.items():
                add(k, v)
        return deps

    def op(self, E, fn, reads=(), writes=(), sig=True):
        self._waits(E, self._deps(reads, writes))
        val = E.n + 1
        if sig:
            sem = self.sems[E.k]
            E.prog.append(lambda h, fn=fn, sem=sem: fn(h).then_inc(sem, 1))
            E.n = val
        else:
            E.prog.append(lambda h, fn=fn: fn(h))
        for b in reads:
            if b.r.get(E.k, 0) < val:
                b.r[E.k] = val
        for b in writes:
            b.w = (E.k, val)
            b.r = {}

    def dma(self, Q, ob, out_ap, ib, in_ap):
        self._waits(Q, self._deps([ib], [ob]))
        ob.dcnt += 16
        sem = self.sems[ob.dsem]
        Q.prog.append(lambda h, o=out_ap, i=in_ap, sem=sem: h.dma_start(out=o, in_=i).then_inc(sem, 16))
        ob.w = (ob.dsem, ob.dcnt)
        ob.r = {}
        if ib.r.get(ob.dsem, 0) < ob.dcnt:
            ib.r[ob.dsem] = ob.dcnt

    def wait_buf(self, E, b):
        self._waits(E, self._deps([b], []))


def alias_barrier(old, new):
    deps = {}
    for b in old:
        if b.w and deps.get(b.w[0], 0) < b.w[1]:
            deps[b.w[0]] = b.w[1]
        for k, v in b.r.items():
            if deps.get(k, 0) < v:
                deps[k] = v
    for b in new:
        b.w = None
        b.r = dict(deps)


def build(SEQ, DEPTH):
    NT = SEQ // TT
    nc = bass.Bass("TRN2", target_bir_lowering=False)
    cx = Ctx(nc)
    PE = cx.add_engine("pe", False)
    ACT = cx.add_engine("act", SELFSYNC)
    DVE = cx.add_engine("dve", SELFSYNC)
    POOL = cx.add_engine("pool", SELFSYNC)
    SP = Eng("sp", -1, False)

    xin = nc.dram_tensor("xT", [D, SEQ], F32, kind="ExternalInput")
    outT = nc.dram_tensor("outT", [D, SEQ], F32, kind="ExternalOutput")
    consts_d = nc.dram_tensor("consts", [128, NCON], F32, kind="ExternalInput")
    params_d = nc.dram_tensor("params", [DEPTH, 128, NPAR], F32, kind="ExternalInput")
    x1 = nc.dram_tensor("x1T", [D, SEQ], F32)
    xmid_d = nc.dram_tensor("xmidT", [D, SEQ], F32)
    wspecs = [("win", 84 * 128, 4096), ("pw", 8 * 128, 1024), ("wout", 32 * 128, 4096),
              ("fin", 172 * 128, 4096), ("fout", 4 * 32 * 128, QKC * 128)]
    wsh, wbf, wg, wgbuf = {}, {}, {}, {}
    for l in range(DEPTH):
        for (nm, rows, C) in wspecs:
            key = (l, nm)
            wsh[key] = nc.dram_tensor(f"{nm}{l}", [rows, C], F32, kind="ExternalInput")
            wg[key] = nc.dram_tensor(f"{nm}{l}_g", [rows, C], BF16)
            wgbuf[key] = cx.dbuf(f"wg_{nm}{l}")
    b_x = [Buf("xin"), cx.dbuf("x1"), cx.dbuf("out")]
    b_xmid = cx.dbuf("xmid")
    b_in = Buf("ext")

    import contextlib
    es = contextlib.ExitStack()

    def sb(name, shape, dt):
        return es.enter_context(nc.sbuf_tensor(name, shape, dt))

    def ps(name, shape, dt=F32):
        return es.enter_context(nc.psum_tensor(name, shape, dt))

    with es:
        R1 = sb("R1", [128, 16384], F32)
        R1b = R1[:].bitcast(BF16)
        hT = sb("hT", [128, KC, TT], BF16)
        mixT = sb("mixT", [128, KC, TT], BF16)
        WS = [sb(f"ws{i}", [128, 4096], BF16) for i in range(3)]
        CON = sb("con", [128, 3352], F32)
        PAR = sb("par", [128, NPAR], F32)
        identb = sb("identb", [128, 128], BF16)
        onesb = sb("onesb", [128, 128], BF16)
        ones32 = sb("ones32", [128, 128], F32)
        esink = sb("esink", [128, NH], F32)
        Sst = sb("Sst", [128, NH, 128], F32)
        Sbf = sb("Sbf", [128, NH, 128], BF16)
        kTh = sb("kTh", [128, NKV, 128 + TT], BF16)
        vtm = sb("vtm", [128, NKV, 5, 128], BF16)
        uhalo = sb("uhalo", [128, 2 * NFB, 2], F32)
        XB = [sb(f"xb{i}", [128, TT], F32) for i in range(2)]
        TMP = sb("TMP", [128, 3076], F32)
        f1 = TMP[:, 0:514]
        f2 = TMP[:, 514:1028]
        f3 = TMP[:, 1028:1540]
        f4 = TMP[:, 1540:2052]
        f5 = TMP[:, 2052:2564]
        rstd_t = TMP[:, 2564:3076]
        tA = TMP[:, 0:768].rearrange("p (b x) -> p b x", b=2)
        tC = TMP[:, 768:1152]
        tB = TMP[:, 1152:1536].bitcast(BF16).rearrange("p (b x) -> p b x", b=2)
        r_sq = TMP[:, 1540:1668]
        yhalo = sb("yhalo", [128, 8, 32], F32)
        sqb = [TMP[:, 0:256].bitcast(BF16)]
        sm = TMP[:, 1668:1676]
        _rr = TMP[:, 2052:3076].bitcast(BF16)
        r_kd, r_vt, r_pt, r_qd = (_rr[:, i * 512:(i + 1) * 512] for i in range(4))
        PSB = [ps(f"psb{i}", [128, 512]) for i in range(8)]

        bR1 = cx.dbuf("R1")
        bhT = Buf("hT")
        bmix = Buf("mixT")
        bWS = [cx.dbuf(f"ws{i}") for i in range(3)]
        bCON = cx.dbuf("con")
        bPAR = cx.dbuf("par")
        bmisc = Buf("misc")
        bS = Buf("S")
        bSbf = Buf("Sbf")
        bkTh = Buf("kTh")
        bvtm = Buf("vtm")
        buh = Buf("uhalo")
        bXB = [cx.dbuf(f"xb{i}") for i in range(2)]
        bf = [Buf(f"f{i}") for i in range(7)]
        btA, btB, btC = [bf[1], bf[2]], [bf[3]], [bf[2], bf[3]]
        brstd = Buf("rstd")
        byh = Buf("yhalo")
        bzc = Buf("zc")
        bsq = [bf[1]]
        bsm = bf[4]
        brk = {n: Buf(n) for n in ("kd", "vt", "pt", "qd")}
        brk["sq"] = bf[4]
        bPS = [Buf(f"ps{i}") for i in range(8)]
        ccsem = cx.new_sem("cc")
        cvsem = cx.new_sem("cv")

        state = {"ws": 0, "psg": 0, "psm": 0, "xb": 0, "sq": 0}

        def next_psg():
            i = state["psg"]
            state["psg"] = (i + 1) % 4
            return i

        def next_psm():
            i = 4 + state["psm"]
            state["psm"] = (state["psm"] + 1) % 4
            return i

        c_dist = CON[:, 0:256].rearrange("p (b q) -> p b q", b=2)
        c_dec = CON[:, 256:1792].rearrange("p (h q) -> p h q", h=NH)
        c_qdec = CON[:, 1792:3328].rearrange("p (h q) -> p h q", h=NH)
        c_kdec = CON[:, 3328:3340]
        c_cdec = CON[:, 3340:3352]
        slopes = _slopes(NH)
        p_g = [PAR[:, i * 32:(i + 1) * 32] for i in range(4)]
        p_dww = PAR[:, 128:376].rearrange("p (b j) -> p b j", b=8)
        p_dwb = PAR[:, 376:384]
        p_lng = PAR[:, 384:392]
        p_lnb = PAR[:, 392:400]
        p_fdw = PAR[:, 400:916].rearrange("p (b j) -> p b j", b=2 * NFB)
        p_fdb = PAR[:, 916:1088]
        p_sink = PAR[:, 1088:1100]

        bidn = cx.dbuf("identb")
        cx.dma(POOL, bidn, identb[:], b_in, consts_d[:, 3352:3480])
        for l in range(DEPTH):
            for (nm, rows, C) in wspecs:
                key = (l, nm)
                for r0 in range(0, rows, 128):
                    for c0 in range(0, C, 1024):
                        c1 = min(C, c0 + 1024)
                        cx.dma(POOL, wgbuf[key], wg[key][r0:r0 + 128, c0:c1], b_in, wsh[key][r0:r0 + 128, c0:c1])
                        if wgbuf[key].dcnt % 512 == 0:
                            cx.wait_buf(POOL, wgbuf[key])
        import os
        KD = os.environ.get("KDEBUG", "")

        class _Early(Exception):
            pass

        def stage(name):
            if KD != name:
                return
            for E in (PE, ACT, DVE):
                if E.n:
                    POOL.prog.append(lambda h, sem=cx.sems[E.k], v=E.n: h.wait_ge(sem, v))
            for r0 in range(0, D, 128):
                cx.dma(POOL, b_x[2], outT[r0:r0 + 128, :], b_in, xin[r0:r0 + 128, :])
            raise _Early()

        cx.dma(SP, bCON, CON[:], b_in, consts_d[:, 0:3352])
        cx.op(DVE, lambda h: h.memset(onesb[:], 1.0), [bidn], [bmisc])
        cx.op(DVE, lambda h: h.memset(ones32[:], 1.0), [], [bmisc])

        def load_w(key, row0, ncols):
            i = state["ws"]
            state["ws"] = (i + 1) % 3
            cx.dma(SP, bWS[i], WS[i][:, 0:ncols], wgbuf[key], wg[key][row0:row0 + 128, 0:ncols])
            return i

        def gemm_block(key, row0, kcs, rhs_ap_fn, rhs_buf, bank=None):
            wi = load_w(key, row0, kcs * 128)
            pb = next_psg() if bank is None else bank
            for kc in range(kcs):
                cx.op(PE, lambda h, kc=kc, wi=wi, pb=pb: h.matmul(
                    PSB[pb][:], lhsT=WS[wi][:, kc * 128:(kc + 1) * 128], rhs=rhs_ap_fn(kc),
                    start=(kc == 0), stop=(kc == kcs - 1)),
                    [bWS[wi], rhs_buf], [bPS[pb]], sig=(kc == kcs - 1))
            return pb

        def rstd_from_bank(pb, n):
            cx.op(DVE, lambda h: h.tensor_scalar(out=rstd_t, in0=PSB[pb][:], scalar1=1.0 / n, scalar2=EPS,
                                                 op0=ALU.mult, op1=ALU.add), [bPS[pb]], [brstd])
            cx.op(ACT, lambda h: h.activation(out=rstd_t, in_=rstd_t, func=AF.Sqrt), [brstd], [brstd])
            cx.op(DVE, lambda h: h.reciprocal(out=rstd_t, in_=rstd_t), [brstd], [brstd])

        def sumsq_blocks(src_ap_fn, src_buf, nblk):
            pb = next_psm()
            for kc in range(nblk):
                si = state["sq"]
                state["sq"] = 0
                cx.op(ACT, lambda h, kc=kc, si=si: h.activation(out=sqb[si], in_=src_ap_fn(kc), func=AF.Square),
                      [src_buf], [bsq[si]])
                cx.op(PE, lambda h, kc=kc, si=si, pb=pb: h.matmul(PSB[pb][:], lhsT=onesb[:], rhs=sqb[si],
                                                                 start=(kc == 0), stop=(kc == nblk - 1)),
                      [bsq[si], bmisc], [bPS[pb]], sig=True)
            return pb

        yT = R1[:].rearrange("p (k t) -> p k t", k=KC)

        def main_body():
          for l in range(DEPTH):
              xsrc, bxs = ([xin, x1][l], b_x[l]) if DEPTH == 2 else (xin, b_x[0])
              if l == DEPTH - 1:
                  xdst, bxd = outT, b_x[2]
              else:
                  xdst, bxd = x1, b_x[1]
              xsrc_v = xsrc.ap().rearrange("(k p) s -> p k s", p=128)
              xdst_v = xdst.ap().rearrange("(k p) s -> p k s", p=128)
              xmid_v = xmid_d.ap().rearrange("(k p) s -> p k s", p=128)
              cx.dma(SP, bPAR, PAR[:], b_in, params_d[l, :, :])
              cx.op(ACT, lambda h: h.activation(out=esink[:], in_=p_sink, func=AF.Exp), [bPAR], [bmisc])
              cx.op(DVE, lambda h: h.memset(Sst[:], 0.0), [], [bS])
              cx.op(DVE, lambda h: h.memset(Sbf[:], 0.0), [], [bSbf])
              cx.op(DVE, lambda h: h.memset(uhalo[:], 0.0), [], [buh])
              cx.op(DVE, lambda h: h.memset(kTh[:, :, 0:128], 0.0), [], [bkTh])
              cx.op(DVE, lambda h: h.memset(vtm[:, :, 0, :], 0.0), [], [bvtm])

              for t in range(NT):
                  tok = slice(t * TT, (t + 1) * TT)
                  for k0 in range(0, KC, 4):
                      cx.dma(SP, bR1, yT[:, k0:k0 + 4, :], bxs, xsrc_v[:, k0:k0 + 4, tok])
                  pb = sumsq_blocks(lambda kc: yT[:, kc, :], bR1, KC)
                  rstd_from_bank(pb, D)
                  for kc in range(KC):
                      cx.op(DVE, lambda h, kc=kc: h.scalar_tensor_tensor(
                          out=hT[:, kc, :], in0=yT[:, kc, :], scalar=p_g[0][:, kc:kc + 1], in1=rstd_t,
                          op0=ALU.mult, op1=ALU.mult), [bR1, bPAR, brstd], [bhT])
                  hrhs = lambda kc: hT[:, kc, :]
                  stage("n1")

                  def slot(i):
                      return R1b[:, i * TT:(i + 1) * TT]
                  for blk in range(20):
                      pb = gemm_block((l, "win"), blk * 128, KC, hrhs, bhT)
                      if 12 <= blk < 16:
                          dst = kTh[:, blk - 12, 128:128 + TT]
                          cx.op(ACT, lambda h, pb=pb, dst=dst: h.activation(out=dst, in_=PSB[pb][:], func=AF.Copy),
                                [bPS[pb]], [bkTh])
                      else:
                          cx.op(ACT, lambda h, pb=pb, blk=blk: h.activation(out=slot(blk), in_=PSB[pb][:], func=AF.Copy),
                                [bPS[pb]], [bR1])
                  stage("attg")
                  for g in range(NKV):
                      pb = next_psm()
                      pbv = PSB[pb][:].bitcast(BF16)
                      for c in range(4):
                          cx.op(PE, lambda h, g=g, c=c, pbv=pbv: h.transpose(
                              pbv[:, c * 128:(c + 1) * 128], slot(16 + g)[:, c * 128:(c + 1) * 128], identb[:]),
                              [bR1, bmisc], [bPS[pb]], sig=(c == 3))
                      cx.op(ACT, lambda h, g=g, pbv=pbv: h.activation(
                          out=vtm[:, g, 1:5, :], in_=pbv[:, 0:512].rearrange("p (c d) -> p c d", c=4), func=AF.Copy),
                          [bPS[pb]], [bvtm])
                  stage("attv")
                  scale = HD ** -0.5
                  for g in range(NKV):
                      for c in range(4):
                          first = (t == 0 and c == 0)
                          qv = R1b[:, 0:NH * TT].rearrange("p (h t) -> p h t", h=NH)[:, 3 * g:3 * g + 3, c * 128:(c + 1) * 128]
                          blocks = [1] if first else [0, 1]
                          pbs = {}
                          for bi in blocks:
                              pb = next_psm()
                              pbs[bi] = pb
                              kap = kTh[:, g, c * 128 + bi * 128: c * 128 + bi * 128 + 128]
                              cx.op(PE, lambda h, pb=pb, kap=kap, qv=qv: h.matmul(
                                  PSB[pb][:, 0:384].rearrange("p (h q) -> p h q", h=3), lhsT=kap, rhs=qv,
                                  start=True, stop=True), [bkTh, bR1], [bPS[pb]])
                          for bi in blocks:
                              pb = pbs[bi]
                              for hh in range(3):
                                  cx.op(DVE, lambda h, pb=pb, bi=bi, g=g, hh=hh: h.scalar_tensor_tensor(
                                      out=tA[:, bi, hh * 128:(hh + 1) * 128], in0=c_dist[:, bi, :],
                                      scalar=-slopes[3 * g + hh] / scale,
                                      in1=PSB[pb][:, hh * 128:(hh + 1) * 128], op0=ALU.mult, op1=ALU.add),
                                      [bPS[pb], bCON], btA)
                          lo = blocks[0]
                          cx.op(ACT, lambda h, lo=lo: h.activation(out=tB[:, lo:2, :], in_=tA[:, lo:2, :], func=AF.Exp, scale=scale),
                                btA, btB)
                          po = next_psm()
                          for j, bi in enumerate(blocks):
                              cx.op(PE, lambda h, po=po, bi=bi, g=g, c=c, j=j, nb=len(blocks): h.matmul(
                                  PSB[po][:, 0:384], lhsT=vtm[:, g, c + bi, :], rhs=tB[:, bi, :],
                                  start=(j == 0), stop=(j == nb - 1)),
                                  [bvtm] + btB, [bPS[po]], sig=(j == len(blocks) - 1))
                          pd = next_psm()
                          for j, bi in enumerate(blocks):
                              cx.op(PE, lambda h, pd=pd, bi=bi, j=j, nb=len(blocks): h.matmul(
                                  PSB[pd][:, 0:384], lhsT=onesb[:], rhs=tB[:, bi, :],
                                  start=(j == 0), stop=(j == nb - 1)),
                                  [bmisc] + btB, [bPS[pd]], sig=(j == len(blocks) - 1))
                          for hh in range(3):
                              cx.op(DVE, lambda h, pd=pd, hh=hh, g=g: h.tensor_scalar(
                                  out=tC[:, hh * 128:(hh + 1) * 128], in0=PSB[pd][:, hh * 128:(hh + 1) * 128],
                                  scalar1=esink[:, 3 * g + hh:3 * g + hh + 1], scalar2=None, op0=ALU.add),
                                  [bPS[pd], bmisc], btC)
                          cx.op(DVE, lambda h: h.reciprocal(out=tC, in_=tC), btC, btC)
                          cx.op(DVE, lambda h, po=po, g=g, c=c: h.tensor_tensor(
                              out=mixT[:, 3 * g:3 * g + 3, c * 128:(c + 1) * 128],
                              in0=PSB[po][:, 0:384].rearrange("p (h q) -> p h q", h=3),
                              in1=tC.rearrange("p (h q) -> p h q", h=3), op=ALU.mult),
                              [bPS[po]] + btC, [bmix])
                  cx.op(DVE, lambda h: h.tensor_copy(out=kTh[:, :, 0:128], in_=kTh[:, :, TT:TT + 128]), [bkTh], [bkTh])
                  cx.op(DVE, lambda h: h.tensor_copy(out=vtm[:, :, 0, :], in_=vtm[:, :, 4, :]), [bvtm], [bvtm])

                  stage("att")
                  yTc = R1[:, 0:8 * 544].rearrange("p (b t) -> p b t", b=8)
                  sTc = R1b[:, 9216:9216 + 8 * TT].rearrange("p (b t) -> p b t", b=8)
                  zc = R1[:, 8192:8192 + 8 * TT].rearrange("p (b t) -> p b t", b=8)
                  alias_barrier([bR1], [bzc])
                  if t == 0:
                      cx.op(DVE, lambda h: h.memset(yTc[:, :, 0:32], 0.0), [], [bR1])
                  else:
                      cx.op(DVE, lambda h: h.tensor_copy(out=yTc[:, :, 0:32], in_=yhalo[:]), [byh], [bR1])
                  ps1 = next_psm()
                  ps2 = next_psm()
                  deferred = []
                  for i in range(8):
                      pa = gemm_block((l, "win"), (20 + i) * 128, KC, hrhs, bhT)
                      pg = gemm_block((l, "win"), (28 + i) * 128, KC, hrhs, bhT)
                      for d_ in deferred:
                          d_()
                      deferred = []
                      cx.op(ACT, lambda h, pg=pg: h.activation(out=f3, in_=PSB[pg][:], func=AF.Sigmoid), [bPS[pg]], [bf[3]])
                      cx.op(DVE, lambda h, pa=pa, i=i: h.tensor_tensor(out=yTc[:, i, 32:544], in0=PSB[pa][:], in1=f3, op=ALU.mult),
                            [bPS[pa], bf[3]], [bR1])
                      cx.op(ACT, lambda h, i=i: h.activation(out=zc[:, i, :], in_=yTc[:, i, 2:2 + TT], func=AF.Identity,
                                                             bias=p_dwb[:, i:i + 1], scale=p_dww[:, i, 0:1]), [bR1, bPAR], [bzc])
                      for j in range(1, CW):
                          cx.op(DVE, lambda h, i=i, j=j: h.scalar_tensor_tensor(out=zc[:, i, :], in0=yTc[:, i, 2 + j:2 + j + TT], scalar=p_dww[:, i, j:j + 1],
                                                                               in1=zc[:, i, :], op0=ALU.mult, op1=ALU.add), [bR1, bPAR, bzc], [bzc])
                      cx.op(ACT, lambda h, i=i: h.activation(out=f4, in_=zc[:, i, :], func=AF.Square), [bzc], [bf[4]])
                      deferred.append(lambda i=i, ps1=ps1: cx.op(PE, lambda h: h.matmul(PSB[ps1][:], lhsT=ones32[:], rhs=zc[:, i, :], start=(i == 0), stop=(i == 7)),
                                                                 [bzc, bmisc], [bPS[ps1]]))
                      deferred.append(lambda i=i, ps2=ps2: cx.op(PE, lambda h: h.matmul(PSB[ps2][:], lhsT=ones32[:], rhs=f4, start=(i == 0), stop=(i == 7)),
                                                                 [bf[4], bmisc], [bPS[ps2]]))
                  for d_ in deferred:
                      d_()
                  cx.op(DVE, lambda h: h.tensor_copy(out=yhalo[:], in_=yTc[:, :, 512:544]), [bR1], [byh])
                  cx.op(DVE, lambda h, ps1=ps1: h.tensor_scalar(out=f4, in0=PSB[ps1][:], scalar1=1.0 / CONVC, scalar2=None, op0=ALU.mult), [bPS[ps1]], [bf[4]])
                  cx.op(DVE, lambda h: h.tensor_tensor(out=f5, in0=f4, in1=f4, op=ALU.mult), [bf[4]], [bf[5]])
                  cx.op(DVE, lambda h, ps2=ps2: h.scalar_tensor_tensor(out=f5, in0=PSB[ps2][:], scalar=1.0 / CONVC, in1=f5, op0=ALU.mult, op1=ALU.subtract),
                        [bPS[ps2], bf[5]], [bf[5]])
                  cx.op(DVE, lambda h: h.tensor_scalar(out=f5, in0=f5, scalar1=EPS, scalar2=None, op0=ALU.add), [bf[5]], [bf[5]])
                  cx.op(ACT, lambda h: h.activation(out=f5, in_=f5, func=AF.Sqrt), [bf[5]], [bf[5]])
                  cx.op(DVE, lambda h: h.reciprocal(out=f5, in_=f5), [bf[5]], [bf[5]])
                  for i in range(8):
                      cx.op(DVE, lambda h, i=i: h.tensor_tensor(out=f3, in0=zc[:, i, :], in1=f4, op=ALU.subtract), [bzc, bf[4]], [bf[3]])
                      cx.op(DVE, lambda h: h.tensor_tensor(out=f3, in0=f3, in1=f5, op=ALU.mult), [bf[3], bf[5]], [bf[3]])
                      cx.op(ACT, lambda h, i=i: h.activation(out=sTc[:, i, :], in_=f3, func=AF.Silu, bias=p_lnb[:, i:i + 1], scale=p_lng[:, i:i + 1]),
                            [bf[3], bPAR], [bR1])
                  for ob in range(8):
                      pb = gemm_block((l, "pw"), ob * 128, 8, lambda kc: sTc[:, kc, :], bR1)
                      cx.op(ACT, lambda h, pb=pb, ob=ob: h.activation(out=mixT[:, 12 + ob, :], in_=PSB[pb][:], func=AF.Copy), [bPS[pb]], [bmix])

                  alias_barrier([bR1, bzc], [bR1])
                  stage("conv")
                  rbufs = [brk[n] for n in ("kd", "vt", "pt", "qd")]
                  alias_barrier([bf[5], brstd], rbufs)
                  bH = [Buf("H0"), Buf("H1")]
                  alias_barrier([bR1], bH)

                  def ret_gemm(half):
                      for hh in range(6):
                          hd = half * 6 + hh
                          for kind, base in enumerate((36, 48, 60, 72)):
                              pb = gemm_block((l, "win"), (base + hd) * 128, KC, hrhs, bhT)
                              fn = AF.Silu if kind == 3 else AF.Copy
                              cx.op(ACT, lambda h, pb=pb, s=24 * half + kind * 6 + hh, fn=fn: h.activation(out=slot(s), in_=PSB[pb][:], func=fn),
                                    [bPS[pb]], [bH[half]])
                              yield

                  def ret_chain(half):
                      bRh = bH[half]
                      for hh in range(6):
                          hd = half * 6 + hh
                          qs, ks, vs, gs = (slot(24 * half + kk * 6 + hh) for kk in range(4))
                          S4 = Sbf[:, 4 * (hd % 3):4 * (hd % 3) + 4, :]
                          pk_ = next_psm()
                          pkv = PSB[pk_][:].bitcast(BF16)
                          for c in range(4):
                              cx.op(PE, lambda h, pkv=pkv, ks=ks, c=c: h.transpose(pkv[:, c * 128:(c + 1) * 128], ks[:, c * 128:(c + 1) * 128], identb[:]),
                                    [bRh, bmisc], [bPS[pk_]], sig=(c == 3))
                          cx.op(DVE, lambda h, pkv=pkv, hd=hd: h.tensor_scalar(out=r_kd, in0=pkv[:, 0:512], scalar1=c_kdec[:, hd:hd + 1], scalar2=None, op0=ALU.mult),
                                [bPS[pk_], bCON], [brk["kd"]])
                          pv_ = next_psm()
                          pvv = PSB[pv_][:].bitcast(BF16)
                          for c in range(4):
                              cx.op(PE, lambda h, pvv=pvv, vs=vs, c=c: h.transpose(pvv[:, c * 128:(c + 1) * 128], vs[:, c * 128:(c + 1) * 128], identb[:]),
                                    [bRh, bmisc], [bPS[pv_]], sig=(c == 3))
                          cx.op(ACT, lambda h, pvv=pvv: h.activation(out=r_vt, in_=pvv[:, 0:512], func=AF.Copy), [bPS[pv_]], [brk["vt"]])
                          yield
                          pkv_ = next_psm()
                          for c in range(4):
                              cx.op(PE, lambda h, pkv_=pkv_, c=c: h.matmul(PSB[pkv_][:, c * 128:(c + 1) * 128], lhsT=r_kd[:, c * 128:(c + 1) * 128],
                                                                           rhs=r_vt[:, c * 128:(c + 1) * 128], start=True, stop=True),
                                    [brk["kd"], brk["vt"]], [bPS[pkv_]], sig=(c == 3))
                          for c in range(4):
                              cx.op(ACT, lambda h, hd=hd, c=c, S4=S4: h.activation(out=S4[:, c, :], in_=Sst[:, hd, :], func=AF.Copy), [bS], [bSbf])
                              cx.op(DVE, lambda h, pkv_=pkv_, hd=hd, c=c: h.scalar_tensor_tensor(out=Sst[:, hd, :], in0=Sst[:, hd, :], scalar=c_cdec[:, hd:hd + 1],
                                                                                            in1=PSB[pkv_][:, c * 128:(c + 1) * 128], op0=ALU.mult, op1=ALU.add),
                                    [bS, bCON, bPS[pkv_]], [bS])
                          pss = next_psm()
                          for c in range(4):
                              cx.op(PE, lambda h, pss=pss, ks=ks, qs=qs, c=c: h.matmul(PSB[pss][:, c * 128:(c + 1) * 128], lhsT=ks[:, c * 128:(c + 1) * 128],
                                                                                       rhs=qs[:, c * 128:(c + 1) * 128], start=True, stop=True),
                                    [bRh], [bPS[pss]], sig=(c == 3))
                          for c in range(4):
                              cx.op(DVE, lambda h, pss=pss, hd=hd, c=c: h.tensor_tensor(out=r_pt[:, c * 128:(c + 1) * 128], in0=PSB[pss][:, c * 128:(c + 1) * 128],
                                                                                        in1=c_dec[:, hd, :], op=ALU.mult), [bPS[pss], bCON], [brk["pt"]])
                              cx.op(DVE, lambda h, qs=qs, hd=hd, c=c: h.tensor_tensor(out=r_qd[:, c * 128:(c + 1) * 128], in0=qs[:, c * 128:(c + 1) * 128],
                                                                                      in1=c_qdec[:, hd, :], op=ALU.mult), [bRh, bCON], [brk["qd"]])
                          yield
                          po = next_psm()
                          for c in range(4):
                              cx.op(PE, lambda h, po=po, c=c: h.matmul(PSB[po][:, c * 128:(c + 1) * 128], lhsT=r_vt[:, c * 128:(c + 1) * 128],
                                                                       rhs=r_pt[:, c * 128:(c + 1) * 128], start=True, stop=False),
                                    [brk["vt"], brk["pt"]], [bPS[po]], sig=False)
                              cx.op(PE, lambda h, po=po, c=c, S4=S4: h.matmul(PSB[po][:, c * 128:(c + 1) * 128], lhsT=S4[:, c, :],
                                                                              rhs=r_qd[:, c * 128:(c + 1) * 128], start=False, stop=True),
                                    [bSbf, brk["qd"]], [bPS[po]], sig=(c == 3))
                          cx.op(ACT, lambda h, po=po: h.activation(out=f1[:, 0:TT], in_=PSB[po][:], func=AF.Copy), [bPS[po]], [bf[1]])
                          cx.op(ACT, lambda h, po=po: h.activation(out=f2[:, 0:TT], in_=PSB[po][:], func=AF.Square), [bPS[po]], [bf[2]])
                          yield
                          pm = next_psm()
                          cx.op(PE, lambda h, pm=pm: h.matmul(PSB[pm][:], lhsT=ones32[:], rhs=f1[:, 0:TT], start=True, stop=True), [bf[1], bmisc], [bPS[pm]])
                          pq2 = next_psm()
                          cx.op(PE, lambda h, pq2=pq2: h.matmul(PSB[pq2][:], lhsT=ones32[:], rhs=f2[:, 0:TT], start=True, stop=True), [bf[2], bmisc], [bPS[pq2]])
                          cx.op(DVE, lambda h, pm=pm: h.tensor_scalar(out=f3, in0=PSB[pm][:], scalar1=1.0 / HD, scalar2=None, op0=ALU.mult), [bPS[pm]], [bf[3]])
                          cx.op(DVE, lambda h: h.tensor_tensor(out=f4, in0=f3, in1=f3, op=ALU.mult), [bf[3]], [bf[4]])
                          cx.op(DVE, lambda h, pq2=pq2: h.scalar_tensor_tensor(out=f4, in0=PSB[pq2][:], scalar=1.0 / HD, in1=f4, op0=ALU.mult, op1=ALU.subtract),
                                [bPS[pq2], bf[4]], [bf[4]])
                          cx.op(DVE, lambda h: h.tensor_scalar(out=f4, in0=f4, scalar1=EPS, scalar2=None, op0=ALU.add), [bf[4]], [bf[4]])
                          cx.op(ACT, lambda h: h.activation(out=f4, in_=f4, func=AF.Sqrt), [bf[4]], [bf[4]])
                          cx.op(DVE, lambda h: h.reciprocal(out=f4, in_=f4), [bf[4]], [bf[4]])
                          cx.op(DVE, lambda h: h.tensor_tensor(out=f1[:, 0:TT], in0=f1[:, 0:TT], in1=f3, op=ALU.subtract), [bf[1], bf[3]], [bf[1]])
                          cx.op(DVE, lambda h: h.tensor_tensor(out=f1[:, 0:TT], in0=f1[:, 0:TT], in1=f4, op=ALU.mult), [bf[1], bf[4]], [bf[1]])
                          cx.op(DVE, lambda h, gs=gs, hd=hd: h.tensor_tensor(out=mixT[:, 20 + hd, :], in0=f1[:, 0:TT], in1=gs, op=ALU.mult), [bf[1], bRh], [bmix])
                          yield

                  for _ in ret_gemm(0):
                      pass
                  ch0 = ret_chain(0)
                  for _ in ret_gemm(1):
                      next(ch0, None)
                  for _ in ch0:
                      pass
                  for _ in ret_chain(1):
                      pass
                  alias_barrier(bH, [bR1])
                  alias_barrier(rbufs, [bf[5], brstd])
                  stage("ret")
                  pstat = next_psm()
                  wdef = []
                  for ob in range(KC):
                      pb = gemm_block((l, "wout"), ob * 128, KC, lambda kc: mixT[:, kc, :], bmix)
                      cx.op(ACT, lambda h, pb=pb, ob=ob: h.activation(out=yT[:, ob, :], in_=PSB[pb][:], func=AF.Copy), [bPS[pb]], [bR1])
                      si = state["sq"]
                      state["sq"] = 0
                      for d_ in wdef:
                          d_()
                      wdef = []
                      cx.op(ACT, lambda h, pb=pb, si=si: h.activation(out=sqb[si], in_=PSB[pb][:], func=AF.Square), [bPS[pb]], [bsq[si]])
                      wdef.append(lambda si=si, ob=ob, pstat=pstat: cx.op(PE, lambda h: h.matmul(PSB[pstat][:], lhsT=onesb[:], rhs=sqb[si], start=(ob == 0), stop=(ob == KC - 1)),
                                                                          [bsq[si], bmisc], [bPS[pstat]]))
                  for d_ in wdef:
                      d_()
                  rstd_from_bank(pstat, D)
                  stage("wout")
                  for ob in range(KC):
                      xi = state["xb"]
                      state["xb"] = (xi + 1) % 2
                      cx.dma(SP, bXB[xi], XB[xi][:], bxs, xsrc_v[:, ob, tok])
                      cx.op(DVE, lambda h, ob=ob: h.tensor_tensor(out=yT[:, ob, :], in0=yT[:, ob, :], in1=rstd_t, op=ALU.mult), [bR1, brstd], [bR1])
                      cx.op(DVE, lambda h, ob=ob, xi=xi: h.scalar_tensor_tensor(out=yT[:, ob, :], in0=yT[:, ob, :], scalar=p_g[1][:, ob:ob + 1], in1=XB[xi][:],
                                                                               op0=ALU.mult, op1=ALU.add), [bR1, bPAR, bXB[xi]], [bR1])
                  for k0 in range(0, KC, 4):
                      cx.dma(SP, b_xmid, xmid_v[:, k0:k0 + 4, tok], bR1, yT[:, k0:k0 + 4, :])

              for t in range(NT):
                  tok = slice(t * TT, (t + 1) * TT)
                  for k0 in range(0, KC, 4):
                      cx.dma(SP, bR1, yT[:, k0:k0 + 4, :], b_xmid, xmid_v[:, k0:k0 + 4, tok])
                  pb = sumsq_blocks(lambda kc: yT[:, kc, :], bR1, KC)
                  rstd_from_bank(pb, D)
                  for kc in range(KC):
                      cx.op(DVE, lambda h, kc=kc: h.scalar_tensor_tensor(out=hT[:, kc, :], in0=yT[:, kc, :], scalar=p_g[2][:, kc:kc + 1], in1=rstd_t,
                                                                        op0=ALU.mult, op1=ALU.mult), [bR1, bPAR, brstd], [bhT])
                  hrhs = lambda kc: hT[:, kc, :]

                  stage("res1")
                  qoff = 0
                  for q in range(4):
                      nq = QSZ[q]
                      for ii in range(nq):
                          i = qoff + ii
                          pg = gemm_block((l, "fin"), i * 128, KC, hrhs, bhT)
                          pv = gemm_block((l, "fin"), (NFB + i) * 128, KC, hrhs, bhT)
                          res = {}
                          for (nm, pbk, bi, fb, bfb) in (("g", pg, i, f1, bf[1]), ("v", pv, NFB + i, f2, bf[2])):
                              cx.op(ACT, lambda h, pbk=pbk, fb=fb: h.activation(out=fb[:, 2:TT + 2], in_=PSB[pbk][:], func=AF.Copy), [bPS[pbk]], [bfb])
                              cx.op(ACT, lambda h, fb=fb, bi=bi: h.activation(out=fb[:, 0:2], in_=uhalo[:, bi, :], func=AF.Copy), [buh], [bfb])
                              cx.op(ACT, lambda h, fb=fb, bi=bi: h.activation(out=uhalo[:, bi, :], in_=fb[:, TT:TT + 2], func=AF.Copy), [bfb], [buh])
                              dst, bdst = (f3, bf[3]) if nm == "g" else (f4, bf[4])
                              cx.op(ACT, lambda h, fb=fb, bi=bi, dst=dst: h.activation(out=dst[:], in_=fb[:, 0:TT], func=AF.Identity,
                                                                                      bias=p_fdb[:, bi:bi + 1], scale=p_fdw[:, bi, 0:1]), [bfb, bPAR], [bdst])
                              cx.op(DVE, lambda h, fb=fb, bi=bi, dst=dst: h.scalar_tensor_tensor(out=dst[:], in0=fb[:, 1:TT + 1], scalar=p_fdw[:, bi, 1:2], in1=dst[:],
                                                                                                op0=ALU.mult, op1=ALU.add), [bfb, bPAR, bdst], [bdst])
                              cx.op(DVE, lambda h, fb=fb, bi=bi, dst=dst: h.scalar_tensor_tensor(out=dst[:], in0=fb[:, 2:TT + 2], scalar=p_fdw[:, bi, 2:3], in1=dst[:],
                                                                                                op0=ALU.mult, op1=ALU.add), [bfb, bPAR, bdst], [bdst])
                          cx.op(DVE, lambda h: h.tensor_tensor(out=f5, in0=f3, in1=f3, op=ALU.mult), [bf[3]], [bf[5]])
                          cx.op(DVE, lambda h: h.tensor_scalar(out=f5, in0=f5, scalar1=0.044715, scalar2=1.0, op0=ALU.mult, op1=ALU.add), [bf[5]], [bf[5]])
                          cx.op(DVE, lambda h: h.tensor_tensor(out=f5, in0=f5, in1=f3, op=ALU.mult), [bf[5], bf[3]], [bf[5]])
                          cx.op(ACT, lambda h: h.activation(out=f5, in_=f5, func=AF.Sigmoid, scale=1.5957691216057308), [bf[5]], [bf[5]])
                          cx.op(DVE, lambda h: h.tensor_tensor(out=f5, in0=f5, in1=f3, op=ALU.mult), [bf[5], bf[3]], [bf[5]])
                          cx.op(DVE, lambda h, ii=ii: h.tensor_tensor(out=mixT[:, ii, :], in0=f5, in1=f4, op=ALU.mult), [bf[5], bf[4]], [bmix])
                      for ob in range(KC):
                          pb = gemm_block((l, "fout"), (q * KC + ob) * 128, nq, lambda kc: mixT[:, kc, :], bmix)
                          if q == 0:
                              cx.op(ACT, lambda h, pb=pb, ob=ob: h.activation(out=yT[:, ob, :], in_=PSB[pb][:], func=AF.Copy), [bPS[pb]], [bR1])
                          else:
                              cx.op(DVE, lambda h, pb=pb, ob=ob: h.tensor_tensor(out=yT[:, ob, :], in0=yT[:, ob, :], in1=PSB[pb][:], op=ALU.add), [bR1, bPS[pb]], [bR1])
                      qoff += nq
                  stage("ffn")
                  pb = sumsq_blocks(lambda kc: yT[:, kc, :], bR1, KC)
                  rstd_from_bank(pb, D)
                  for ob in range(KC):
                      xi = state["xb"]
                      state["xb"] = (xi + 1) % 2
                      cx.dma(SP, bXB[xi], XB[xi][:], b_xmid, xmid_v[:, ob, tok])
                      cx.op(DVE, lambda h, ob=ob: h.tensor_tensor(out=yT[:, ob, :], in0=yT[:, ob, :], in1=rstd_t, op=ALU.mult), [bR1, brstd], [bR1])
                      cx.op(DVE, lambda h, ob=ob, xi=xi: h.scalar_tensor_tensor(out=yT[:, ob, :], in0=yT[:, ob, :], scalar=p_g[3][:, ob:ob + 1], in1=XB[xi][:],
                                                                               op0=ALU.mult, op1=ALU.add), [bR1, bPAR, bXB[xi]], [bR1])
                  for k0 in range(0, KC, 4):
                      cx.dma(SP, bxd, xdst_v[:, k0:k0 + 4, tok], bR1, yT[:, k0:k0 + 4, :])
        try:
            main_body()
        except _Early:
            pass
        cx.wait_buf(POOL, b_x[2])

        with nc.Block() as block:
            @block.tensor
            def _(h):
                for f in PE.prog:
                    f(h)

            @block.scalar
            def _(h):
                for f in ACT.prog:
                    f(h)

            @block.vector
            def _(h):
                for f in DVE.prog:
                    f(h)

            @block.gpsimd
            def _(h):
                for f in POOL.prog:
                    f(h)

            @block.sync
            def _(h):
                for f in SP.prog:
                    f(h)
    return nc


def _tile_w(W):
    K, N = W.shape
    return np.ascontiguousarray(W.reshape(K // 128, 128, N // 128, 128).transpose(2, 1, 0, 3)).reshape(N // 128 * 128, K)


def _consts():
    c = np.zeros((128, NCON), np.float32)
    j = np.arange(128)[:, None].astype(np.float64)
    i = np.arange(128)[None, :].astype(np.float64)
    dp = i + 128 - j
    dc = i - j
    c[:, 0:128] = np.where(dp < 128, dp, 1e9)
    c[:, 128:256] = np.where(dc >= 0, dc, 1e9)
    lg = np.log1p(-np.exp2(-5.0 - np.arange(NH, dtype=np.float64)))
    dec = np.zeros((128, NH, 128), np.float64)
    qdec = np.zeros((128, NH, 128), np.float64)
    for h in range(NH):
        rel = i - j
        dec[:, h, :] = np.where(rel >= 0, np.exp(lg[h] * np.maximum(rel, 0.0)), 0.0) * (HD ** -0.5)
        qdec[:, h, :] = np.exp(lg[h] * (i + 1.0))
    c[:, 256:1792] = dec.reshape(128, -1)
    c[:, 1792:3328] = qdec.reshape(128, -1)
    c[:, 3328:3340] = np.exp(lg[None, :] * (127.0 - j)) * (HD ** -0.5)
    c[:, 3340:3352] = np.exp(lg * 128.0)[None, :]
    c[:, 3352:3480] = np.eye(128, dtype=np.float32)
    return c


def _fm(v, nb):
    return np.ascontiguousarray(np.asarray(v).reshape(nb, 128).T)


_CACHE = {}


def kernel(x, mix_pre_g, mix_post_g, ffn_pre_g, ffn_post_g, w_in, att_sinks, conv_dw_w, conv_dw_b,
           conv_ln_g, conv_ln_b, conv_pw_w, w_out, ffn_w_in, ffn_dw_w, ffn_dw_b, ffn_w_out):
    x = np.asarray(x, np.float32)
    B, SEQ, _ = x.shape
    DEPTH = np.asarray(w_in).shape[0]
    kk = (SEQ, DEPTH)
    if kk not in _CACHE:
        _CACHE[kk] = build(SEQ, DEPTH)
    nc = _CACHE[kk]
    consts = _consts()
    params = np.zeros((DEPTH, 128, NPAR), np.float32)
    shards = [dict() for _ in range(NCORE)]
    for l in range(DEPTH):
        p = params[l]
        p[:, 0:32] = _fm(mix_pre_g[l], 32)
        p[:, 32:64] = _fm(mix_post_g[l], 32)
        p[:, 64:96] = _fm(ffn_pre_g[l], 32)
        p[:, 96:128] = _fm(ffn_post_g[l], 32)
        dww = np.asarray(conv_dw_w[l])
        p[:, 128:376] = dww.reshape(CW, 8, 128).transpose(2, 1, 0).reshape(128, -1)
        p[:, 376:384] = _fm(conv_dw_b[l], 8)
        p[:, 384:392] = _fm(conv_ln_g[l], 8)
        p[:, 392:400] = _fm(conv_ln_b[l], 8)
        fdw = np.asarray(ffn_dw_w[l])
        p[:, 400:916] = fdw.reshape(3, 2 * NFB, 128).transpose(2, 1, 0).reshape(128, -1)
        p[:, 916:1088] = _fm(ffn_dw_b[l], 2 * NFB)
        p[:, 1088:1100] = np.asarray(att_sinks[l])[None, :]
        fo = np.asarray(ffn_w_out[l], np.float32)
        fparts = []
        r0 = 0
        for q in range(4):
            kq = QSZ[q] * 128
            tq = _tile_w(fo[r0:r0 + kq])
            if kq < QKC * 128:
                tq = np.concatenate([tq, np.zeros((tq.shape[0], QKC * 128 - kq), np.float32)], axis=1)
            fparts.append(tq)
            r0 += kq
        tiled = {"win": _tile_w(np.asarray(w_in[l], np.float32)), "pw": _tile_w(np.asarray(conv_pw_w[l], np.float32)),
                 "wout": _tile_w(np.asarray(w_out[l], np.float32)), "fin": _tile_w(np.asarray(ffn_w_in[l], np.float32)),
                 "fout": np.concatenate(fparts, axis=0)}
        for nm, arr in tiled.items():
            for c in range(NCORE):
                shards[c][f"{nm}{l}"] = arr
        del tiled, fparts, fo
    in_maps = []
    ncore = B
    for c in range(ncore):
        m = dict(shards[c])
        m["xT"] = np.ascontiguousarray(x[c % B].T)
        m["consts"] = consts
        m["params"] = params
        in_maps.append(m)
    res = run_bass_kernel_spmd(nc, in_maps, core_ids=list(range(ncore)))
    out = np.stack([np.ascontiguousarray(res.results[b]["outT"].T) for b in range(B)], axis=0)
    return out.astype(np.float32)
```

```python
import math
import numpy as np
import concourse.bass as bass
import concourse.mybir as mybir
from concourse.bass_utils import run_bass_kernel_spmd

F32 = mybir.dt.float32
BF16 = mybir.dt.bfloat16
AF = mybir.ActivationFunctionType
ALU = mybir.AluOpType
AX = mybir.AxisListType

D = 4096
KC = 32
NCORE = 8
TT = 512
HD = 128
NH = 12
NKV = 4
CONVC = 1024
CW = 31
DFF = 11008
NFB = 86
QSZ = [22, 22, 21, 21]
QKC = 22
EPS = 1e-6
NEG = -30000.0
NPAR = 1100
NCON = 3480
SELFSYNC = True


def _slopes(n):
    def p2(n):
        s = 2.0 ** (-8.0 / n)
        return [s ** (i + 1) for i in range(n)]
    if math.log2(n).is_integer():
        return p2(n)
    c = 2 ** math.floor(math.log2(n))
    return p2(c) + _slopes(2 * c)[0::2][: n - c]


class Buf:
    def __init__(self, name, dsem=None):
        self.name = name
        self.w = None
        self.r = {}
        self.dsem = dsem
        self.dcnt = 0


class Eng:
    def __init__(self, name, k, selfsync):
        self.name = name
        self.k = k
        self.n = 0
        self.waited = {}
        self.selfsync = selfsync
        self.prog = []


class Ctx:
    def __init__(self, nc):
        self.nc = nc
        self.sems = []
        self.owner = {}
        self.engs = {}

    def new_sem(self, name):
        s = self.nc.alloc_semaphore(name)
        self.sems.append(s)
        return len(self.sems) - 1

    def add_engine(self, name, selfsync):
        k = self.new_sem("p_" + name)
        e = Eng(name, k, selfsync)
        self.owner[k] = e
        self.engs[name] = e
        return e

    def dbuf(self, name):
        return Buf(name, self.new_sem("d_" + name))

    def _waits(self, E, deps):
        for k, v in deps.items():
            if k == E.k and not E.selfsync:
                continue
            if k in self.owner:
                assert v <= self.owner[k].n, ("dep on unsignalled", E.name, self.owner[k].name, v)
            if E.waited.get(k, 0) < v:
                sem = self.sems[k]
                E.prog.append(lambda h, sem=sem, v=v: h.wait_ge(sem, v))
                E.waited[k] = v

    @staticmethod
    def _deps(reads, writes):
        deps = {}

        def add(k, v):
            if deps.get(k, 0) < v:
                deps[k] = v
        for b in reads:
            if b.w:
                add(*b.w)
        for b in writes:
            if b.w:
                add(*b.w)
            for k, v in b.r.items():
                add(k, v)
        return deps

    def op(self, E, fn, reads=(), writes=(), sig=True):
        self._waits(E, self._deps(reads, writes))
        val = E.n + 1
        if sig:
            sem = self.sems[E.k]
            E.prog.append(lambda h, fn=fn, sem=sem: fn(h).then_inc(sem, 1))
            E.n = val
        else:
            E.prog.append(lambda h, fn=fn: fn(h))
        for b in reads:
            if b.r.get(E.k, 0) < val:
                b.r[E.k] = val
        for b in writes:
            b.w = (E.k, val)
            b.r = {}

    def dma(self, Q, ob, out_ap, ib, in_ap):
        self._waits(Q, self._deps([ib], [ob]))
        ob.dcnt += 16
        sem = self.sems[ob.dsem]
        Q.prog.append(lambda h, o=out_ap, i=in_ap, sem=sem: h.dma_start(out=o, in_=i).then_inc(sem, 16))
        ob.w = (ob.dsem, ob.dcnt)
        ob.r = {}
        if ib.r.get(ob.dsem, 0) < ob.dcnt:
            ib.r[ob.dsem] = ob.dcnt

    def wait_buf(self, E, b):
        self._waits(E, self._deps([b], []))


def alias_barrier(old, new):
    deps = {}
    for b in old:
        if b.w and deps.get(b.w[0], 0) < b.w[1]:
            deps[b.w[0]] = b.w[1]
        for k, v in b.r.items():
            if deps.get(k, 0) < v:
                deps[k] = v
    for b in new:
        b.w = None
        b.r = dict(deps)


def build(SEQ, DEPTH):
    NT = SEQ // TT
    nc = bass.Bass("TRN2", target_bir_lowering=False)
    cx = Ctx(nc)
    PE = cx.add_engine("pe", False)
    ACT = cx.add_engine("act", SELFSYNC)
    DVE = cx.add_engine("dve", SELFSYNC)
    POOL = cx.add_engine("pool", SELFSYNC)
    SP = Eng("sp", -1, False)

    xin = nc.dram_tensor("xT", [D, SEQ], F32, kind="ExternalInput")
    outT = nc.dram_tensor("outT", [D, SEQ], F32, kind="ExternalOutput")
    consts_d = nc.dram_tensor("consts", [128, NCON], F32, kind="ExternalInput")
    params_d = nc.dram_tensor("params", [DEPTH, 128, NPAR], F32, kind="ExternalInput")
    x1 = nc.dram_tensor("x1T", [D, SEQ], F32)
    xmid_d = nc.dram_tensor("xmidT", [D, SEQ], F32)
    wspecs = [("win", 84 * 128, 4096), ("pw", 8 * 128, 1024), ("wout", 32 * 128, 4096),
              ("fin", 172 * 128, 4096), ("fout", 4 * 32 * 128, QKC * 128)]
    wsh, wbf, wg, wgbuf = {}, {}, {}, {}
    for l in range(DEPTH):
        for (nm, rows, C) in wspecs:
            key = (l, nm)
            wsh[key] = nc.dram_tensor(f"{nm}{l}", [rows, C], F32, kind="ExternalInput")
            wg[key] = nc.dram_tensor(f"{nm}{l}_g", [rows, C], BF16)
            wgbuf[key] = cx.dbuf(f"wg_{nm}{l}")
    WG = 14
    wgw = {l: [cx.dbuf(f"wgwin{l}_{g}") for g in range(84 // WG)] for l in range(DEPTH)}

    def wbuf_of(key, row0):
        if key[1] == "win":
            return wgw[key[0]][row0 // 128 // WG]
        return wgbuf[key]
    b_x = [Buf("xin"), cx.dbuf("x1"), cx.dbuf("out")]
    b_xmid = cx.dbuf("xmid")
    b_in = Buf("ext")

    import contextlib
    es = contextlib.ExitStack()

    def sb(name, shape, dt):
        return es.enter_context(nc.sbuf_tensor(name, shape, dt))

    def ps(name, shape, dt=F32):
        return es.enter_context(nc.psum_tensor(name, shape, dt))

    with es:
        R1 = sb("R1", [128, 16384], F32)
        R1b = R1[:].bitcast(BF16)
        hT = sb("hT", [128, KC, TT], BF16)
        mixT = sb("mixT", [128, KC, TT], BF16)
        WS = [sb(f"ws{i}", [128, 4096], BF16) for i in range(3)]
        CON = sb("con", [128, 3352], F32)
        PAR = sb("par", [128, NPAR], F32)
        identb = sb("identb", [128, 128], BF16)
        onesb = sb("onesb", [128, 128], BF16)
        ones32 = sb("ones32", [128, 128], F32)
        esink = sb("esink", [128, NH], F32)
        Sst = sb("Sst", [128, NH, 128], F32)
        Sbf = sb("Sbf", [128, NH, 128], BF16)
        kTh = sb("kTh", [128, NKV, 128 + TT], BF16)
        vtm = sb("vtm", [128, NKV, 5, 128], BF16)
        uhalo = sb("uhalo", [128, 2 * NFB, 2], F32)
        XB = [sb(f"xb{i}", [128, TT], F32) for i in range(2)]
        TMP = sb("TMP", [128, 3076], F32)
        f1 = TMP[:, 0:514]
        f2 = TMP[:, 514:1028]
        f3 = TMP[:, 1028:1540]
        f4 = TMP[:, 1540:2052]
        f5 = TMP[:, 2052:2564]
        rstd_t = TMP[:, 2564:3076]
        tA = TMP[:, 0:768].rearrange("p (b x) -> p b x", b=2)
        tC = TMP[:, 768:1152]
        tB = TMP[:, 1152:1536].bitcast(BF16).rearrange("p (b x) -> p b x", b=2)
        r_sq = TMP[:, 1540:1668]
        yhalo = sb("yhalo", [128, 8, 32], F32)
        sqb = [TMP[:, 0:256].bitcast(BF16)]
        sm = TMP[:, 1668:1676]
        _rr = TMP[:, 2052:3076].bitcast(BF16)
        r_kd, r_vt, r_pt, r_qd = (_rr[:, i * 512:(i + 1) * 512] for i in range(4))
        PSB = [ps(f"psb{i}", [128, 512]) for i in range(8)]

        bR1 = cx.dbuf("R1")
        bhT = Buf("hT")
        bmix = Buf("mixT")
        bWS = [cx.dbuf(f"ws{i}") for i in range(3)]
        bCON = cx.dbuf("con")
        bPAR = cx.dbuf("par")
        bmisc = Buf("misc")
        bS = Buf("S")
        bSbf = Buf("Sbf")
        bkTh = Buf("kTh")
        bvtm = Buf("vtm")
        buh = Buf("uhalo")
        bXB = [cx.dbuf(f"xb{i}") for i in range(2)]
        bf = [Buf(f"f{i}") for i in range(7)]
        btA, btB, btC = [bf[1], bf[2]], [bf[3]], [bf[2], bf[3]]
        brstd = Buf("rstd")
        byh = Buf("yhalo")
        bzc = Buf("zc")
        bsq = [bf[1]]
        bsm = bf[4]
        brk = {n: Buf(n) for n in ("kd", "vt", "pt", "qd")}
        brk["sq"] = bf[4]
        bPS = [Buf(f"ps{i}") for i in range(8)]
        ccsem = cx.new_sem("cc")
        cvsem = cx.new_sem("cv")

        state = {"ws": 0, "psg": 0, "psm": 0, "xb": 0, "sq": 0}

        def next_psg():
            i = state["psg"]
            state["psg"] = (i + 1) % 4
            return i

        def next_psm():
            i = 4 + state["psm"]
            state["psm"] = (state["psm"] + 1) % 4
            return i

        c_dist = CON[:, 0:256].rearrange("p (b q) -> p b q", b=2)
        c_dec = CON[:, 256:1792].rearrange("p (h q) -> p h q", h=NH)
        c_qdec = CON[:, 1792:3328].rearrange("p (h q) -> p h q", h=NH)
        c_kdec = CON[:, 3328:3340]
        c_cdec = CON[:, 3340:3352]
        slopes = _slopes(NH)
        p_g = [PAR[:, i * 32:(i + 1) * 32] for i in range(4)]
        p_dww = PAR[:, 128:376].rearrange("p (b j) -> p b j", b=8)
        p_dwb = PAR[:, 376:384]
        p_lng = PAR[:, 384:392]
        p_lnb = PAR[:, 392:400]
        p_fdw = PAR[:, 400:916].rearrange("p (b j) -> p b j", b=2 * NFB)
        p_fdb = PAR[:, 916:1088]
        p_sink = PAR[:, 1088:1100]

        bidn = cx.dbuf("identb")
        cx.dma(POOL, bidn, identb[:], b_in, consts_d[:, 3352:3480])
        for l in range(DEPTH):
            for (nm, rows, C) in wspecs:
                key = (l, nm)
                for r0 in range(0, rows, 128):
                    for c0 in range(0, C, 1024):
                        c1 = min(C, c0 + 1024)
                        wb_ = wbuf_of(key, r0)
                        cx.dma(POOL, wb_, wg[key][r0:r0 + 128, c0:c1], b_in, wsh[key][r0:r0 + 128, c0:c1])
                        if wb_.dcnt % 512 == 0:
                            cx.wait_buf(POOL, wb_)
        import os
        KD = os.environ.get("KDEBUG", "")

        class _Early(Exception):
            pass

        def stage(name):
            if KD != name:
                return
            for E in (PE, ACT, DVE):
                if E.n:
                    POOL.prog.append(lambda h, sem=cx.sems[E.k], v=E.n: h.wait_ge(sem, v))
            for r0 in range(0, D, 128):
                cx.dma(POOL, b_x[2], outT[r0:r0 + 128, :], b_in, xin[r0:r0 + 128, :])
            raise _Early()

        cx.dma(SP, bCON, CON[:], b_in, consts_d[:, 0:3352])
        cx.op(DVE, lambda h: h.memset(onesb[:], 1.0), [bidn], [bmisc])
        cx.op(DVE, lambda h: h.memset(ones32[:], 1.0), [], [bmisc])

        def load_w(key, row0, ncols):
            i = state["ws"]
            state["ws"] = (i + 1) % 3
            cx.dma(SP, bWS[i], WS[i][:, 0:ncols], wbuf_of(key, row0), wg[key][row0:row0 + 128, 0:ncols])
            return i

        def gemm_block(key, row0, kcs, rhs_ap_fn, rhs_buf, bank=None):
            wi = load_w(key, row0, kcs * 128)
            pb = next_psg() if bank is None else bank
            for kc in range(kcs):
                cx.op(PE, lambda h, kc=kc, wi=wi, pb=pb: h.matmul(
                    PSB[pb][:], lhsT=WS[wi][:, kc * 128:(kc + 1) * 128], rhs=rhs_ap_fn(kc),
                    start=(kc == 0), stop=(kc == kcs - 1)),
                    [bWS[wi], rhs_buf], [bPS[pb]], sig=(kc == kcs - 1))
            return pb

        def rstd_from_bank(pb, n):
            cx.op(DVE, lambda h: h.tensor_scalar(out=rstd_t, in0=PSB[pb][:], scalar1=1.0 / n, scalar2=EPS,
                                                 op0=ALU.mult, op1=ALU.add), [bPS[pb]], [brstd])
            cx.op(ACT, lambda h: h.activation(out=rstd_t, in_=rstd_t, func=AF.Sqrt), [brstd], [brstd])
            cx.op(DVE, lambda h: h.reciprocal(out=rstd_t, in_=rstd_t), [brstd], [brstd])

        def sumsq_blocks(src_ap_fn, src_buf, nblk):
            pb = next_psm()
            for kc in range(nblk):
                si = state["sq"]
                state["sq"] = 0
                cx.op(ACT, lambda h, kc=kc, si=si: h.activation(out=sqb[si], in_=src_ap_fn(kc), func=AF.Square),
                      [src_buf], [bsq[si]])
                cx.op(PE, lambda h, kc=kc, si=si, pb=pb: h.matmul(PSB[pb][:], lhsT=onesb[:], rhs=sqb[si],
                                                                 start=(kc == 0), stop=(kc == nblk - 1)),
                      [bsq[si], bmisc], [bPS[pb]], sig=True)
            return pb

        yT = R1[:].rearrange("p (k t) -> p k t", k=KC)

        def main_body():
          for l in range(DEPTH):
              xsrc, bxs = ([xin, x1][l], b_x[l]) if DEPTH == 2 else (xin, b_x[0])
              if l == DEPTH - 1:
                  xdst, bxd = outT, b_x[2]
              else:
                  xdst, bxd = x1, b_x[1]
              xsrc_v = xsrc.ap().rearrange("(k p) s -> p k s", p=128)
              xdst_v = xdst.ap().rearrange("(k p) s -> p k s", p=128)
              xmid_v = xmid_d.ap().rearrange("(k p) s -> p k s", p=128)
              cx.dma(SP, bPAR, PAR[:], b_in, params_d[l, :, :])
              cx.op(ACT, lambda h: h.activation(out=esink[:], in_=p_sink, func=AF.Exp), [bPAR], [bmisc])
              cx.op(DVE, lambda h: h.memset(Sst[:], 0.0), [], [bS])
              cx.op(DVE, lambda h: h.memset(Sbf[:], 0.0), [], [bSbf])
              cx.op(DVE, lambda h: h.memset(uhalo[:], 0.0), [], [buh])
              cx.op(DVE, lambda h: h.memset(kTh[:, :, 0:128], 0.0), [], [bkTh])
              cx.op(DVE, lambda h: h.memset(vtm[:, :, 0, :], 0.0), [], [bvtm])

              for t in range(NT):
                  tok = slice(t * TT, (t + 1) * TT)
                  for k0 in range(0, KC, 4):
                      cx.dma(SP, bR1, yT[:, k0:k0 + 4, :], bxs, xsrc_v[:, k0:k0 + 4, tok])
                  pb = sumsq_blocks(lambda kc: yT[:, kc, :], bR1, KC)
                  rstd_from_bank(pb, D)
                  for kc in range(KC):
                      cx.op(DVE, lambda h, kc=kc: h.scalar_tensor_tensor(
                          out=hT[:, kc, :], in0=yT[:, kc, :], scalar=p_g[0][:, kc:kc + 1], in1=rstd_t,
                          op0=ALU.mult, op1=ALU.mult), [bR1, bPAR, brstd], [bhT])
                  hrhs = lambda kc: hT[:, kc, :]
                  stage("n1")

                  def slot(i):
                      return R1b[:, i * TT:(i + 1) * TT]
                  for blk in range(20):
                      pb = gemm_block((l, "win"), blk * 128, KC, hrhs, bhT)
                      if 12 <= blk < 16:
                          dst = kTh[:, blk - 12, 128:128 + TT]
                          cx.op(ACT, lambda h, pb=pb, dst=dst: h.activation(out=dst, in_=PSB[pb][:], func=AF.Copy),
                                [bPS[pb]], [bkTh])
                      else:
                          cx.op(ACT, lambda h, pb=pb, blk=blk: h.activation(out=slot(blk), in_=PSB[pb][:], func=AF.Copy),
                                [bPS[pb]], [bR1])
                  stage("attg")
                  for g in range(NKV):
                      pb = next_psm()
                      pbv = PSB[pb][:].bitcast(BF16)
                      for c in range(4):
                          cx.op(PE, lambda h, g=g, c=c, pbv=pbv: h.transpose(
                              pbv[:, c * 128:(c + 1) * 128], slot(16 + g)[:, c * 128:(c + 1) * 128], identb[:]),
                              [bR1, bmisc], [bPS[pb]], sig=(c == 3))
                      cx.op(ACT, lambda h, g=g, pbv=pbv: h.activation(
                          out=vtm[:, g, 1:5, :], in_=pbv[:, 0:512].rearrange("p (c d) -> p c d", c=4), func=AF.Copy),
                          [bPS[pb]], [bvtm])
                  stage("attv")
                  scale = HD ** -0.5
                  for g in range(NKV):
                      for c in range(4):
                          first = (t == 0 and c == 0)
                          qv = R1b[:, 0:NH * TT].rearrange("p (h t) -> p h t", h=NH)[:, 3 * g:3 * g + 3, c * 128:(c + 1) * 128]
                          blocks = [1] if first else [0, 1]
                          pbs = {}
                          for bi in blocks:
                              pb = next_psm()
                              pbs[bi] = pb
                              kap = kTh[:, g, c * 128 + bi * 128: c * 128 + bi * 128 + 128]
                              cx.op(PE, lambda h, pb=pb, kap=kap, qv=qv: h.matmul(
                                  PSB[pb][:, 0:384].rearrange("p (h q) -> p h q", h=3), lhsT=kap, rhs=qv,
                                  start=True, stop=True), [bkTh, bR1], [bPS[pb]])
                          for bi in blocks:
                              pb = pbs[bi]
                              for hh in range(3):
                                  cx.op(DVE, lambda h, pb=pb, bi=bi, g=g, hh=hh: h.scalar_tensor_tensor(
                                      out=tA[:, bi, hh * 128:(hh + 1) * 128], in0=c_dist[:, bi, :],
                                      scalar=-slopes[3 * g + hh] / scale,
                                      in1=PSB[pb][:, hh * 128:(hh + 1) * 128], op0=ALU.mult, op1=ALU.add),
                                      [bPS[pb], bCON], btA)
                          lo = blocks[0]
                          cx.op(ACT, lambda h, lo=lo: h.activation(out=tB[:, lo:2, :], in_=tA[:, lo:2, :], func=AF.Exp, scale=scale),
                                btA, btB)
                          po = next_psm()
                          for j, bi in enumerate(blocks):
                              cx.op(PE, lambda h, po=po, bi=bi, g=g, c=c, j=j, nb=len(blocks): h.matmul(
                                  PSB[po][:, 0:384], lhsT=vtm[:, g, c + bi, :], rhs=tB[:, bi, :],
                                  start=(j == 0), stop=(j == nb - 1)),
                                  [bvtm] + btB, [bPS[po]], sig=(j == len(blocks) - 1))
                          pd = next_psm()
                          for j, bi in enumerate(blocks):
                              cx.op(PE, lambda h, pd=pd, bi=bi, j=j, nb=len(blocks): h.matmul(
                                  PSB[pd][:, 0:384], lhsT=onesb[:], rhs=tB[:, bi, :],
                                  start=(j == 0), stop=(j == nb - 1)),
                                  [bmisc] + btB, [bPS[pd]], sig=(j == len(blocks) - 1))
                          for hh in range(3):
                              cx.op(DVE, lambda h, pd=pd, hh=hh, g=g: h.tensor_scalar(
                                  out=tC[:, hh * 128:(hh + 1) * 128], in0=PSB[pd][:, hh * 128:(hh + 1) * 128],
                                  scalar1=esink[:, 3 * g + hh:3 * g + hh + 1], scalar2=None, op0=ALU.add),
                                  [bPS[pd], bmisc], btC)
                          cx.op(DVE, lambda h: h.reciprocal(out=tC, in_=tC), btC, btC)
                          cx.op(DVE, lambda h, po=po, g=g, c=c: h.tensor_tensor(
                              out=mixT[:, 3 * g:3 * g + 3, c * 128:(c + 1) * 128],
                              in0=PSB[po][:, 0:384].rearrange("p (h q) -> p h q", h=3),
                              in1=tC.rearrange("p (h q) -> p h q", h=3), op=ALU.mult),
                              [bPS[po]] + btC, [bmix])
                  cx.op(DVE, lambda h: h.tensor_copy(out=kTh[:, :, 0:128], in_=kTh[:, :, TT:TT + 128]), [bkTh], [bkTh])
                  cx.op(DVE, lambda h: h.tensor_copy(out=vtm[:, :, 0, :], in_=vtm[:, :, 4, :]), [bvtm], [bvtm])

                  stage("att")
                  yTc = R1[:, 0:8 * 544].rearrange("p (b t) -> p b t", b=8)
                  sTc = R1b[:, 9216:9216 + 8 * TT].rearrange("p (b t) -> p b t", b=8)
                  zc = R1[:, 8192:8192 + 8 * TT].rearrange("p (b t) -> p b t", b=8)
                  alias_barrier([bR1], [bzc])
                  if t == 0:
                      cx.op(DVE, lambda h: h.memset(yTc[:, :, 0:32], 0.0), [], [bR1])
                  else:
                      cx.op(DVE, lambda h: h.tensor_copy(out=yTc[:, :, 0:32], in_=yhalo[:]), [byh], [bR1])
                  ps1 = next_psm()
                  ps2 = next_psm()
                  deferred = []
                  for i in range(8):
                      pa = gemm_block((l, "win"), (20 + i) * 128, KC, hrhs, bhT)
                      pg = gemm_block((l, "win"), (28 + i) * 128, KC, hrhs, bhT)
                      for d_ in deferred:
                          d_()
                      deferred = []
                      cx.op(ACT, lambda h, pg=pg: h.activation(out=f3, in_=PSB[pg][:], func=AF.Sigmoid), [bPS[pg]], [bf[3]])
                      cx.op(DVE, lambda h, pa=pa, i=i: h.tensor_tensor(out=yTc[:, i, 32:544], in0=PSB[pa][:], in1=f3, op=ALU.mult),
                            [bPS[pa], bf[3]], [bR1])
                      cx.op(ACT, lambda h, i=i: h.activation(out=zc[:, i, :], in_=yTc[:, i, 2:2 + TT], func=AF.Identity,
                                                             bias=p_dwb[:, i:i + 1], scale=p_dww[:, i, 0:1]), [bR1, bPAR], [bzc])
                      for j in range(1, CW):
                          cx.op(DVE, lambda h, i=i, j=j: h.scalar_tensor_tensor(out=zc[:, i, :], in0=yTc[:, i, 2 + j:2 + j + TT], scalar=p_dww[:, i, j:j + 1],
                                                                               in1=zc[:, i, :], op0=ALU.mult, op1=ALU.add), [bR1, bPAR, bzc], [bzc])
                      cx.op(ACT, lambda h, i=i: h.activation(out=f4, in_=zc[:, i, :], func=AF.Square), [bzc], [bf[4]])
                      deferred.append(lambda i=i, ps1=ps1: cx.op(PE, lambda h: h.matmul(PSB[ps1][:], lhsT=ones32[:], rhs=zc[:, i, :], start=(i == 0), stop=(i == 7)),
                                                                 [bzc, bmisc], [bPS[ps1]]))
                      deferred.append(lambda i=i, ps2=ps2: cx.op(PE, lambda h: h.matmul(PSB[ps2][:], lhsT=ones32[:], rhs=f4, start=(i == 0), stop=(i == 7)),
                                                                 [bf[4], bmisc], [bPS[ps2]]))
                  for d_ in deferred:
                      d_()
                  cx.op(DVE, lambda h: h.tensor_copy(out=yhalo[:], in_=yTc[:, :, 512:544]), [bR1], [byh])
                  cx.op(DVE, lambda h, ps1=ps1: h.tensor_scalar(out=f4, in0=PSB[ps1][:], scalar1=1.0 / CONVC, scalar2=None, op0=ALU.mult), [bPS[ps1]], [bf[4]])
                  cx.op(DVE, lambda h: h.tensor_tensor(out=f5, in0=f4, in1=f4, op=ALU.mult), [bf[4]], [bf[5]])
                  cx.op(DVE, lambda h, ps2=ps2: h.scalar_tensor_tensor(out=f5, in0=PSB[ps2][:], scalar=1.0 / CONVC, in1=f5, op0=ALU.mult, op1=ALU.subtract),
                        [bPS[ps2], bf[5]], [bf[5]])
                  cx.op(DVE, lambda h: h.tensor_scalar(out=f5, in0=f5, scalar1=EPS, scalar2=None, op0=ALU.add), [bf[5]], [bf[5]])
                  cx.op(ACT, lambda h: h.activation(out=f5, in_=f5, func=AF.Sqrt), [bf[5]], [bf[5]])
                  cx.op(DVE, lambda h: h.reciprocal(out=f5, in_=f5), [bf[5]], [bf[5]])
                  for i in range(8):
                      cx.op(DVE, lambda h, i=i: h.tensor_tensor(out=f3, in0=zc[:, i, :], in1=f4, op=ALU.subtract), [bzc, bf[4]], [bf[3]])
                      cx.op(DVE, lambda h: h.tensor_tensor(out=f3, in0=f3, in1=f5, op=ALU.mult), [bf[3], bf[5]], [bf[3]])
                      cx.op(ACT, lambda h, i=i: h.activation(out=sTc[:, i, :], in_=f3, func=AF.Silu, bias=p_lnb[:, i:i + 1], scale=p_lng[:, i:i + 1]),
                            [bf[3], bPAR], [bR1])
                  for ob in range(8):
                      pb = gemm_block((l, "pw"), ob * 128, 8, lambda kc: sTc[:, kc, :], bR1)
                      cx.op(ACT, lambda h, pb=pb, ob=ob: h.activation(out=mixT[:, 12 + ob, :], in_=PSB[pb][:], func=AF.Copy), [bPS[pb]], [bmix])

                  alias_barrier([bR1, bzc], [bR1])
                  stage("conv")
                  rbufs = [brk[n] for n in ("kd", "vt", "pt", "qd")]
                  alias_barrier([bf[5], brstd], rbufs)
                  bH = [Buf("H0"), Buf("H1")]
                  alias_barrier([bR1], bH)

                  def ret_gemm(half):
                      for hh in range(6):
                          hd = half * 6 + hh
                          for kind, base in enumerate((36, 48, 60, 72)):
                              pb = gemm_block((l, "win"), (base + hd) * 128, KC, hrhs, bhT)
                              fn = AF.Silu if kind == 3 else AF.Copy
                              cx.op(ACT, lambda h, pb=pb, s=24 * half + kind * 6 + hh, fn=fn: h.activation(out=slot(s), in_=PSB[pb][:], func=fn),
                                    [bPS[pb]], [bH[half]])
                              yield

                  def ret_chain(half):
                      bRh = bH[half]
                      for hh in range(6):
                          hd = half * 6 + hh
                          qs, ks, vs, gs = (slot(24 * half + kk * 6 + hh) for kk in range(4))
                          S4 = Sbf[:, 4 * (hd % 3):4 * (hd % 3) + 4, :]
                          pk_ = next_psm()
                          pkv = PSB[pk_][:].bitcast(BF16)
                          for c in range(4):
                              cx.op(PE, lambda h, pkv=pkv, ks=ks, c=c: h.transpose(pkv[:, c * 128:(c + 1) * 128], ks[:, c * 128:(c + 1) * 128], identb[:]),
                                    [bRh, bmisc], [bPS[pk_]], sig=(c == 3))
                          cx.op(DVE, lambda h, pkv=pkv, hd=hd: h.tensor_scalar(out=r_kd, in0=pkv[:, 0:512], scalar1=c_kdec[:, hd:hd + 1], scalar2=None, op0=ALU.mult),
                                [bPS[pk_], bCON], [brk["kd"]])
                          pv_ = next_psm()
                          pvv = PSB[pv_][:].bitcast(BF16)
                          for c in range(4):
                              cx.op(PE, lambda h, pvv=pvv, vs=vs, c=c: h.transpose(pvv[:, c * 128:(c + 1) * 128], vs[:, c * 128:(c + 1) * 128], identb[:]),
                                    [bRh, bmisc], [bPS[pv_]], sig=(c == 3))
                          cx.op(ACT, lambda h, pvv=pvv: h.activation(out=r_vt, in_=pvv[:, 0:512], func=AF.Copy), [bPS[pv_]], [brk["vt"]])
                          yield
                          pkv_ = next_psm()
                          for c in range(4):
                              cx.op(PE, lambda h, pkv_=pkv_, c=c: h.matmul(PSB[pkv_][:, c * 128:(c + 1) * 128], lhsT=r_kd[:, c * 128:(c + 1) * 128],
                                                                           rhs=r_vt[:, c * 128:(c + 1) * 128], start=True, stop=True),
                                    [brk["kd"], brk["vt"]], [bPS[pkv_]], sig=(c == 3))
                          for c in range(4):
                              cx.op(ACT, lambda h, hd=hd, c=c, S4=S4: h.activation(out=S4[:, c, :], in_=Sst[:, hd, :], func=AF.Copy), [bS], [bSbf])
                              cx.op(DVE, lambda h, pkv_=pkv_, hd=hd, c=c: h.scalar_tensor_tensor(out=Sst[:, hd, :], in0=Sst[:, hd, :], scalar=c_cdec[:, hd:hd + 1],
                                                                                            in1=PSB[pkv_][:, c * 128:(c + 1) * 128], op0=ALU.mult, op1=ALU.add),
                                    [bS, bCON, bPS[pkv_]], [bS])
                          pss = next_psm()
                          for c in range(4):
                              cx.op(PE, lambda h, pss=pss, ks=ks, qs=qs, c=c: h.matmul(PSB[pss][:, c * 128:(c + 1) * 128], lhsT=ks[:, c * 128:(c + 1) * 128],
                                                                                       rhs=qs[:, c * 128:(c + 1) * 128], start=True, stop=True),
                                    [bRh], [bPS[pss]], sig=(c == 3))
                          for c in range(4):
                              cx.op(DVE, lambda h, pss=pss, hd=hd, c=c: h.tensor_tensor(out=r_pt[:, c * 128:(c + 1) * 128], in0=PSB[pss][:, c * 128:(c + 1) * 128],
                                                                                        in1=c_dec[:, hd, :], op=ALU.mult), [bPS[pss], bCON], [brk["pt"]])
                              cx.op(DVE, lambda h, qs=qs, hd=hd, c=c: h.tensor_tensor(out=r_qd[:, c * 128:(c + 1) * 128], in0=qs[:, c * 128:(c + 1) * 128],
                                                                                      in1=c_qdec[:, hd, :], op=ALU.mult), [bRh, bCON], [brk["qd"]])
                          yield
                          po = next_psm()
                          for c in range(4):
                              cx.op(PE, lambda h, po=po, c=c: h.matmul(PSB[po][:, c * 128:(c + 1) * 128], lhsT=r_vt[:, c * 128:(c + 1) * 128],
                                                                       rhs=r_pt[:, c * 128:(c + 1) * 128], start=True, stop=False),
                                    [brk["vt"], brk["pt"]], [bPS[po]], sig=False)
                              cx.op(PE, lambda h, po=po, c=c, S4=S4: h.matmul(PSB[po][:, c * 128:(c + 1) * 128], lhsT=S4[:, c, :],
                                                                              rhs=r_qd[:, c * 128:(c + 1) * 128], start=False, stop=True),
                                    [bSbf, brk["qd"]], [bPS[po]], sig=(c == 3))
                          cx.op(ACT, lambda h, po=po: h.activation(out=f1[:, 0:TT], in_=PSB[po][:], func=AF.Copy), [bPS[po]], [bf[1]])
                          cx.op(ACT, lambda h, po=po: h.activation(out=f2[:, 0:TT], in_=PSB[po][:], func=AF.Square), [bPS[po]], [bf[2]])
                          yield
                          pm = next_psm()
                          cx.op(PE, lambda h, pm=pm: h.matmul(PSB[pm][:], lhsT=ones32[:], rhs=f1[:, 0:TT], start=True, stop=True), [bf[1], bmisc], [bPS[pm]])
                          pq2 = next_psm()
                          cx.op(PE, lambda h, pq2=pq2: h.matmul(PSB[pq2][:], lhsT=ones32[:], rhs=f2[:, 0:TT], start=True, stop=True), [bf[2], bmisc], [bPS[pq2]])
                          cx.op(DVE, lambda h, pm=pm: h.tensor_scalar(out=f3, in0=PSB[pm][:], scalar1=1.0 / HD, scalar2=None, op0=ALU.mult), [bPS[pm]], [bf[3]])
                          cx.op(DVE, lambda h: h.tensor_tensor(out=f4, in0=f3, in1=f3, op=ALU.mult), [bf[3]], [bf[4]])
                          cx.op(DVE, lambda h, pq2=pq2: h.scalar_tensor_tensor(out=f4, in0=PSB[pq2][:], scalar=1.0 / HD, in1=f4, op0=ALU.mult, op1=ALU.subtract),
                                [bPS[pq2], bf[4]], [bf[4]])
                          cx.op(DVE, lambda h: h.tensor_scalar(out=f4, in0=f4, scalar1=EPS, scalar2=None, op0=ALU.add), [bf[4]], [bf[4]])
                          cx.op(ACT, lambda h: h.activation(out=f4, in_=f4, func=AF.Sqrt), [bf[4]], [bf[4]])
                          cx.op(DVE, lambda h: h.reciprocal(out=f4, in_=f4), [bf[4]], [bf[4]])
                          cx.op(DVE, lambda h: h.tensor_tensor(out=f1[:, 0:TT], in0=f1[:, 0:TT], in1=f3, op=ALU.subtract), [bf[1], bf[3]], [bf[1]])
                          cx.op(DVE, lambda h: h.tensor_tensor(out=f1[:, 0:TT], in0=f1[:, 0:TT], in1=f4, op=ALU.mult), [bf[1], bf[4]], [bf[1]])
                          cx.op(DVE, lambda h, gs=gs, hd=hd: h.tensor_tensor(out=mixT[:, 20 + hd, :], in0=f1[:, 0:TT], in1=gs, op=ALU.mult), [bf[1], bRh], [bmix])
                          yield

                  for _ in ret_gemm(0):
                      pass
                  ch0 = ret_chain(0)
                  for _ in ret_gemm(1):
                      next(ch0, None)
                  for _ in ch0:
                      pass
                  for _ in ret_chain(1):
                      pass
                  alias_barrier(bH, [bR1])
                  alias_barrier(rbufs, [bf[5], brstd])
                  stage("ret")
                  pstat = next_psm()
                  wdef = []
                  for ob in range(KC):
                      pb = gemm_block((l, "wout"), ob * 128, KC, lambda kc: mixT[:, kc, :], bmix)
                      cx.op(ACT, lambda h, pb=pb, ob=ob: h.activation(out=yT[:, ob, :], in_=PSB[pb][:], func=AF.Copy), [bPS[pb]], [bR1])
                      si = state["sq"]
                      state["sq"] = 0
                      for d_ in wdef:
                          d_()
                      wdef = []
                      cx.op(ACT, lambda h, pb=pb, si=si: h.activation(out=sqb[si], in_=PSB[pb][:], func=AF.Square), [bPS[pb]], [bsq[si]])
                      wdef.append(lambda si=si, ob=ob, pstat=pstat: cx.op(PE, lambda h: h.matmul(PSB[pstat][:], lhsT=onesb[:], rhs=sqb[si], start=(ob == 0), stop=(ob == KC - 1)),
                                                                          [bsq[si], bmisc], [bPS[pstat]]))
                  for d_ in wdef:
                      d_()
                  rstd_from_bank(pstat, D)
                  stage("wout")
                  for ob in range(KC):
                      xi = state["xb"]
                      state["xb"] = (xi + 1) % 2
                      cx.dma(SP, bXB[xi], XB[xi][:], bxs, xsrc_v[:, ob, tok])
                      cx.op(DVE, lambda h, ob=ob: h.tensor_tensor(out=yT[:, ob, :], in0=yT[:, ob, :], in1=rstd_t, op=ALU.mult), [bR1, brstd], [bR1])
                      cx.op(DVE, lambda h, ob=ob, xi=xi: h.scalar_tensor_tensor(out=yT[:, ob, :], in0=yT[:, ob, :], scalar=p_g[1][:, ob:ob + 1], in1=XB[xi][:],
                                                                               op0=ALU.mult, op1=ALU.add), [bR1, bPAR, bXB[xi]], [bR1])
                  for k0 in range(0, KC, 4):
                      cx.dma(SP, b_xmid, xmid_v[:, k0:k0 + 4, tok], bR1, yT[:, k0:k0 + 4, :])

              for t in range(NT):
                  tok = slice(t * TT, (t + 1) * TT)
                  for k0 in range(0, KC, 4):
                      cx.dma(SP, bR1, yT[:, k0:k0 + 4, :], b_xmid, xmid_v[:, k0:k0 + 4, tok])
                  pb = sumsq_blocks(lambda kc: yT[:, kc, :], bR1, KC)
                  rstd_from_bank(pb, D)
                  for kc in range(KC):
                      cx.op(DVE, lambda h, kc=kc: h.scalar_tensor_tensor(out=hT[:, kc, :], in0=yT[:, kc, :], scalar=p_g[2][:, kc:kc + 1], in1=rstd_t,
                                                                        op0=ALU.mult, op1=ALU.mult), [bR1, bPAR, brstd], [bhT])
                  hrhs = lambda kc: hT[:, kc, :]

                  stage("res1")
                  qoff = 0
                  for q in range(4):
                      nq = QSZ[q]
                      for ii in range(nq):
                          i = qoff + ii
                          pg = gemm_block((l, "fin"), i * 128, KC, hrhs, bhT)
                          pv = gemm_block((l, "fin"), (NFB + i) * 128, KC, hrhs, bhT)
                          res = {}
                          for (nm, pbk, bi, fb, bfb) in (("g", pg, i, f1, bf[1]), ("v", pv, NFB + i, f2, bf[2])):
                              cx.op(ACT, lambda h, pbk=pbk, fb=fb: h.activation(out=fb[:, 2:TT + 2], in_=PSB[pbk][:], func=AF.Copy), [bPS[pbk]], [bfb])
                              cx.op(ACT, lambda h, fb=fb, bi=bi: h.activation(out=fb[:, 0:2], in_=uhalo[:, bi, :], func=AF.Copy), [buh], [bfb])
                              cx.op(ACT, lambda h, fb=fb, bi=bi: h.activation(out=uhalo[:, bi, :], in_=fb[:, TT:TT + 2], func=AF.Copy), [bfb], [buh])
                              dst, bdst = (f3, bf[3]) if nm == "g" else (f4, bf[4])
                              cx.op(ACT, lambda h, fb=fb, bi=bi, dst=dst: h.activation(out=dst[:], in_=fb[:, 0:TT], func=AF.Identity,
                                                                                      bias=p_fdb[:, bi:bi + 1], scale=p_fdw[:, bi, 0:1]), [bfb, bPAR], [bdst])
                              cx.op(DVE, lambda h, fb=fb, bi=bi, dst=dst: h.scalar_tensor_tensor(out=dst[:], in0=fb[:, 1:TT + 1], scalar=p_fdw[:, bi, 1:2], in1=dst[:],
                                                                                                op0=ALU.mult, op1=ALU.add), [bfb, bPAR, bdst], [bdst])
                              cx.op(DVE, lambda h, fb=fb, bi=bi, dst=dst: h.scalar_tensor_tensor(out=dst[:], in0=fb[:, 2:TT + 2], scalar=p_fdw[:, bi, 2:3], in1=dst[:],
                                                                                                op0=ALU.mult, op1=ALU.add), [bfb, bPAR, bdst], [bdst])
                          cx.op(DVE, lambda h: h.tensor_tensor(out=f5, in0=f3, in1=f3, op=ALU.mult), [bf[3]], [bf[5]])
                          cx.op(DVE, lambda h: h.tensor_scalar(out=f5, in0=f5, scalar1=0.044715, scalar2=1.0, op0=ALU.mult, op1=ALU.add), [bf[5]], [bf[5]])
                          cx.op(DVE, lambda h: h.tensor_tensor(out=f5, in0=f5, in1=f3, op=ALU.mult), [bf[5], bf[3]], [bf[5]])
                          cx.op(ACT, lambda h: h.activation(out=f5, in_=f5, func=AF.Sigmoid, scale=1.5957691216057308), [bf[5]], [bf[5]])
                          cx.op(DVE, lambda h: h.tensor_tensor(out=f5, in0=f5, in1=f3, op=ALU.mult), [bf[5], bf[3]], [bf[5]])
                          cx.op(DVE, lambda h, ii=ii: h.tensor_tensor(out=mixT[:, ii, :], in0=f5, in1=f4, op=ALU.mult), [bf[5], bf[4]], [bmix])
                      for ob in range(KC):
                          pb = gemm_block((l, "fout"), (q * KC + ob) * 128, nq, lambda kc: mixT[:, kc, :], bmix)
                          if q == 0:
                              cx.op(ACT, lambda h, pb=pb, ob=ob: h.activation(out=yT[:, ob, :], in_=PSB[pb][:], func=AF.Copy), [bPS[pb]], [bR1])
                          else:
                              cx.op(DVE, lambda h, pb=pb, ob=ob: h.tensor_tensor(out=yT[:, ob, :], in0=yT[:, ob, :], in1=PSB[pb][:], op=ALU.add), [bR1, bPS[pb]], [bR1])
                      qoff += nq
                  stage("ffn")
                  pb = sumsq_blocks(lambda kc: yT[:, kc, :], bR1, KC)
                  rstd_from_bank(pb, D)
                  for ob in range(KC):
                      xi = state["xb"]
                      state["xb"] = (xi + 1) % 2
                      cx.dma(SP, bXB[xi], XB[xi][:], b_xmid, xmid_v[:, ob, tok])
                      cx.op(DVE, lambda h, ob=ob: h.tensor_tensor(out=yT[:, ob, :], in0=yT[:, ob, :], in1=rstd_t, op=ALU.mult), [bR1, brstd], [bR1])
                      cx.op(DVE, lambda h, ob=ob, xi=xi: h.scalar_tensor_tensor(out=yT[:, ob, :], in0=yT[:, ob, :], scalar=p_g[3][:, ob:ob + 1], in1=XB[xi][:],
                                                                               op0=ALU.mult, op1=ALU.add), [bR1, bPAR, bXB[xi]], [bR1])
                  for k0 in range(0, KC, 4):
                      cx.dma(SP, bxd, xdst_v[:, k0:k0 + 4, tok], bR1, yT[:, k0:k0 + 4, :])
        try:
            main_body()
        except _Early:
            pass
        cx.wait_buf(POOL, b_x[2])

        with nc.Block() as block:
            @block.tensor
            def _(h):
                for f in PE.prog:
                    f(h)

            @block.scalar
            def _(h):
                for f in ACT.prog:
                    f(h)

            @block.vector
            def _(h):
                for f in DVE.prog:
                    f(h)

            @block.gpsimd
            def _(h):
                for f in POOL.prog:
                    f(h)

            @block.sync
            def _(h):
                for f in SP.prog:
                    f(h)
    return nc


def _tile_w(W):
    K, N = W.shape
    return np.ascontiguousarray(W.reshape(K // 128, 128, N // 128, 128).transpose(2, 1, 0, 3)).reshape(N // 128 * 128, K)


def _consts():
    c = np.zeros((128, NCON), np.float32)
    j = np.arange(128)[:, None].astype(np.float64)
    i = np.arange(128)[None, :].astype(np.float64)
    dp = i + 128 - j
    dc = i - j
    c[:, 0:128] = np.where(dp < 128, dp, 1e9)
    c[:, 128:256] = np.where(dc >= 0, dc, 1e9)
    lg = np.log1p(-np.exp2(-5.0 - np.arange(NH, dtype=np.float64)))
    dec = np.zeros((128, NH, 128), np.float64)
    qdec = np.zeros((128, NH, 128), np.float64)
    for h in range(NH):
        rel = i - j
        dec[:, h, :] = np.where(rel >= 0, np.exp(lg[h] * np.maximum(rel, 0.0)), 0.0) * (HD ** -0.5)
        qdec[:, h, :] = np.exp(lg[h] * (i + 1.0))
    c[:, 256:1792] = dec.reshape(128, -1)
    c[:, 1792:3328] = qdec.reshape(128, -1)
    c[:, 3328:3340] = np.exp(lg[None, :] * (127.0 - j)) * (HD ** -0.5)
    c[:, 3340:3352] = np.exp(lg * 128.0)[None, :]
    c[:, 3352:3480] = np.eye(128, dtype=np.float32)
    return c


def _fm(v, nb):
    return np.ascontiguousarray(np.asarray(v).reshape(nb, 128).T)


_CACHE = {}


def kernel(x, mix_pre_g, mix_post_g, ffn_pre_g, ffn_post_g, w_in, att_sinks, conv_dw_w, conv_dw_b,
           conv_ln_g, conv_ln_b, conv_pw_w, w_out, ffn_w_in, ffn_dw_w, ffn_dw_b, ffn_w_out):
    x = np.asarray(x, np.float32)
    B, SEQ, _ = x.shape
    DEPTH = np.asarray(w_in).shape[0]
    kk = (SEQ, DEPTH)
    if kk not in _CACHE:
        _CACHE[kk] = build(SEQ, DEPTH)
    nc = _CACHE[kk]
    consts = _consts()
    params = np.zeros((DEPTH, 128, NPAR), np.float32)
    shards = [dict() for _ in range(NCORE)]
    for l in range(DEPTH):
        p = params[l]
        p[:, 0:32] = _fm(mix_pre_g[l], 32)
        p[:, 32:64] = _fm(mix_post_g[l], 32)
        p[:, 64:96] = _fm(ffn_pre_g[l], 32)
        p[:, 96:128] = _fm(ffn_post_g[l], 32)
        dww = np.asarray(conv_dw_w[l])
        p[:, 128:376] = dww.reshape(CW, 8, 128).transpose(2, 1, 0).reshape(128, -1)
        p[:, 376:384] = _fm(conv_dw_b[l], 8)
        p[:, 384:392] = _fm(conv_ln_g[l], 8)
        p[:, 392:400] = _fm(conv_ln_b[l], 8)
        fdw = np.asarray(ffn_dw_w[l])
        p[:, 400:916] = fdw.reshape(3, 2 * NFB, 128).transpose(2, 1, 0).reshape(128, -1)
        p[:, 916:1088] = _fm(ffn_dw_b[l], 2 * NFB)
        p[:, 1088:1100] = np.asarray(att_sinks[l])[None, :]
        fo = np.asarray(ffn_w_out[l], np.float32)
        fparts = []
        r0 = 0
        for q in range(4):
            kq = QSZ[q] * 128
            tq = _tile_w(fo[r0:r0 + kq])
            if kq < QKC * 128:
                tq = np.concatenate([tq, np.zeros((tq.shape[0], QKC * 128 - kq), np.float32)], axis=1)
            fparts.append(tq)
            r0 += kq
        tiled = {"win": _tile_w(np.asarray(w_in[l], np.float32)), "pw": _tile_w(np.asarray(conv_pw_w[l], np.float32)),
                 "wout": _tile_w(np.asarray(w_out[l], np.float32)), "fin": _tile_w(np.asarray(ffn_w_in[l], np.float32)),
                 "fout": np.concatenate(fparts, axis=0)}
        for nm, arr in tiled.items():
            for c in range(NCORE):
                shards[c][f"{nm}{l}"] = arr
        del tiled, fparts, fo
    in_maps = []
    ncore = B
    for c in range(ncore):
        m = dict(shards[c])
        m["xT"] = np.ascontiguousarray(x[c % B].T)
        m["consts"] = consts
        m["params"] = params
        in_maps.append(m)
    res = run_bass_kernel_spmd(nc, in_maps, core_ids=list(range(ncore)))
    out = np.stack([np.ascontiguousarray(res.results[b]["outT"].T) for b in range(B)], axis=0)
    return out.astype(np.float32)
```
